# Optimizing a Trainium2 kernel written in Bass

```python
import math
import jax
import jax.numpy as jnp
from jax import lax
import numpy as np

D_MODEL = 1024
BATCH = 16
SEQ = 4096
DEPTH = 4

N_MIXERS = 3
N_A = (DEPTH + N_MIXERS - 1) // N_MIXERS
N_B = (DEPTH + N_MIXERS - 2) // N_MIXERS
N_C = (DEPTH + N_MIXERS - 3) // N_MIXERS
MEM_LEN = 256
DEEPNORM_ALPHA = (2 * DEPTH) ** 0.25
DEEPNORM_BETA = (8 * DEPTH) ** -0.25
NORM_EPS = 1e-5
ROPE_THETA = 10000.0
MAX_POS_OFFSET = 1024
MASK_VALUE = -1e30
MIN_FORGET = 1e-6

A_EXPAND = 128
A_HEADS = D_MODEL // A_EXPAND
A_HEAD_V = D_MODEL // A_HEADS
A_CHUNK = 64

B_GROUPS = ((128, 1), (512, 4), (2048, 16))
B_HEAD_DIM = 64
B_HEADS = D_MODEL // B_HEAD_DIM
B_BLOCK = 128

C_GROUP_CH = 16
C_GROUPS = D_MODEL // C_GROUP_CH
C_STATE = 64
DT_MIN = 1e-3
DT_MAX = 1e-1

M_HEADS = 4
M_HEAD_DIM = D_MODEL // M_HEADS

N_EXPERTS = 32
TOP_K = 4
D_EXPERT = D_MODEL
SWIGLU_ALPHA = 1.702
SWIGLU_LIMIT = 7.0
MOE_BLOCK = 128

kernel_name = 'hybrid_hgrn2_dilated_s5_moe_trunk'


def layer_norm(x, g, b):
    xf = x.astype(jnp.float32)
    mu = jnp.mean(xf, axis=-1, keepdims=True)
    var = jnp.mean(jnp.square(xf - mu), axis=-1, keepdims=True)
    return ((xf - mu) * lax.rsqrt(var + NORM_EPS) * g + b).astype(x.dtype)


def rms_norm(x, g):
    xf = x.astype(jnp.float32)
    y = xf * lax.rsqrt(jnp.mean(jnp.square(xf), axis=-1, keepdims=True) + NORM_EPS)
    return (y * g).astype(x.dtype)


def rope(x, positions):
    half = x.shape[-1] // 2
    inv_freq = ROPE_THETA ** (-jnp.arange(half, dtype=jnp.float32) / half)
    ang = positions.astype(jnp.float32)[..., None] * inv_freq
    cos, sin = jnp.cos(ang)[:, :, None, :], jnp.sin(ang)[:, :, None, :]
    xf = x.astype(jnp.float32)
    x1, x2 = xf[..., :half], xf[..., half:]
    return jnp.concatenate([x1 * cos - x2 * sin, x1 * sin + x2 * cos], axis=-1).astype(x.dtype)


def hgrn2_mixer(x, w_in, norm_g, w_out, lower_bound):
    bsz, seq, dm = x.shape
    n_chunks = seq // A_CHUNK
    q, f, i, g = jnp.split(x @ w_in, 4, axis=-1)
    q = jax.nn.silu(q.astype(jnp.float32))
    lb = lower_bound.astype(jnp.float32)
    fg = lb + (1.0 - lb) * jax.nn.sigmoid(f.astype(jnp.float32))
    log_f = jnp.log(jnp.maximum(fg, MIN_FORGET))
    k = 1.0 - fg

    def to_chunks(t):
        return t.reshape(bsz, n_chunks, A_CHUNK, A_HEADS, -1).transpose(1, 0, 3, 2, 4)

    qc, kc, vc = to_chunks(q), to_chunks(k), to_chunks(i.astype(jnp.float32))
    bc = jnp.cumsum(to_chunks(log_f), axis=3)
    causal = jnp.tril(jnp.ones((A_CHUNK, A_CHUNK), dtype=bool))[:, :, None]

    def chunk_step(state, inp):
        q_c, k_c, v_c, b_c = inp
        rel = b_c[:, :, :, None, :] - b_c[:, :, None, :, :]
        decay = jnp.where(causal, jnp.exp(jnp.where(causal, rel, 0.0)), 0.0)
        scores = jnp.einsum('bhtd,bhsd,bhtsd->bhts', q_c, k_c, decay)
        o = (jnp.einsum('bhts,bhsv->bhtv', scores, v_c)
             + jnp.einsum('bhtd,bhdv->bhtv', q_c * jnp.exp(b_c), state))
        b_end = b_c[:, :, -1:, :]
        new_state = (jnp.exp(b_end[:, :, 0, :, None]) * state
                     + jnp.einsum('bhsd,bhsv->bhdv', k_c * jnp.exp(b_end - b_c), v_c))
        return new_state, o

    state0 = jnp.zeros((bsz, A_HEADS, A_EXPAND, A_HEAD_V), jnp.float32)
    _, o = lax.scan(chunk_step, state0, (qc, kc, vc, bc))
    o = o.transpose(1, 0, 3, 2, 4).reshape(bsz, seq, A_HEADS, A_HEAD_V)
    o = rms_norm(o, norm_g.reshape(A_HEADS, A_HEAD_V))
    o = o * jax.nn.silu(g.astype(jnp.float32)).reshape(bsz, seq, A_HEADS, A_HEAD_V)
    return o.reshape(bsz, seq, dm).astype(x.dtype) @ w_out


def dilated_window_attention(q, k, v, window, dilation):
    bsz, seq, nh, dh = q.shape
    sub_len = seq // dilation
    reach = window // dilation
    sub_pad = -(-sub_len // B_BLOCK) * B_BLOCK
    nb = sub_pad // B_BLOCK
    z = bsz * dilation

    def to_blocks(t):
        t = t.reshape(bsz, sub_len, dilation, nh, dh).transpose(0, 2, 1, 3, 4).reshape(z, sub_len, nh, dh)
        t = jnp.pad(t, ((0, 0), (0, sub_pad - sub_len), (0, 0), (0, 0)))
        return t.reshape(z, nb, B_BLOCK, nh, dh)

    def with_prev(t):
        prev = jnp.pad(t, ((0, 0), (1, 0), (0, 0), (0, 0), (0, 0)))[:, :-1]
        return jnp.concatenate([prev, t], axis=2)

    qb = to_blocks(q)
    kk, vv = with_prev(to_blocks(k)), with_prev(to_blocks(v))
    scores = jnp.einsum('znqhd,znkhd->znhqk', qb, kk).astype(jnp.float32) * (dh ** -0.5)
    q_pos = jnp.arange(nb)[:, None] * B_BLOCK + jnp.arange(B_BLOCK)[None, :]
    k_pos = jnp.arange(nb)[:, None] * B_BLOCK - B_BLOCK + jnp.arange(2 * B_BLOCK)[None, :]
    dist = q_pos[:, :, None] - k_pos[:, None, :]
    valid = (dist >= 0) & (dist <= reach) & (k_pos[:, None, :] >= 0)
    scores = jnp.where(valid[None, :, None], scores, MASK_VALUE)
    lse = jax.nn.logsumexp(scores, axis=-1)
    p = jnp.exp(scores - lse[..., None])
    out = jnp.einsum('znhqk,znkhd->znqhd', p, vv.astype(jnp.float32))

    def from_blocks(t):
        tail = t.shape[3:]
        t = t.reshape(z, sub_pad, *tail)[:, :sub_len]
        t = t.reshape(bsz, dilation, sub_len, *tail)
        return jnp.swapaxes(t, 1, 2).reshape(bsz, seq, *tail)

    return from_blocks(out), from_blocks(jnp.swapaxes(lse, 2, 3)[..., None])[..., 0]


def dilated_mixer(x, positions, w_in, w_out):
    bsz, seq, dm = x.shape
    proj = (x @ w_in).reshape(bsz, seq, len(B_GROUPS), 3, B_HEADS, B_HEAD_DIM)
    outs, lses = [], []
    for gi, (window, dilation) in enumerate(B_GROUPS):
        q = rope(proj[:, :, gi, 0], positions)
        k = rope(proj[:, :, gi, 1], positions)
        o, l = dilated_window_attention(q, k, proj[:, :, gi, 2], window, dilation)
        outs.append(o)
        lses.append(l)
    wts = jax.nn.softmax(jnp.stack(lses, axis=0), axis=0)[..., None]
    o = jnp.sum(wts * jnp.stack(outs, axis=0), axis=0)
    return o.reshape(bsz, seq, dm).astype(x.dtype) @ w_out


def s5_mixer(x, a_re, a_im, log_dt, b_re, b_im, c_re, c_im, d_skip, w_glu):
    bsz, seq, dm = x.shape
    f32 = jnp.float32
    u = x.astype(f32).reshape(bsz, seq, C_GROUPS, C_GROUP_CH)
    ar, ai = a_re.astype(f32), a_im.astype(f32)
    dt = jnp.exp(log_dt.astype(f32))[:, None]
    mag = jnp.exp(ar * dt)
    lam_re, lam_im = mag * jnp.cos(ai * dt), mag * jnp.sin(ai * dt)
    den = ar * ar + ai * ai
    fr = ((lam_re - 1.0) * ar + lam_im * ai) / den
    fi = (lam_im * ar - (lam_re - 1.0) * ai) / den
    br, bi = b_re.astype(f32), b_im.astype(f32)
    bb_re = fr[..., None] * br - fi[..., None] * bi
    bb_im = fr[..., None] * bi + fi[..., None] * br
    bu_re = jnp.einsum('bsgc,gnc->bsgn', u, bb_re)
    bu_im = jnp.einsum('bsgc,gnc->bsgn', u, bb_im)
    la_re = jnp.broadcast_to(lam_re[None, None], (1, seq, C_GROUPS, C_STATE))
    la_im = jnp.broadcast_to(lam_im[None, None], (1, seq, C_GROUPS, C_STATE))

    def combine(left, right):
        a1r, a1i, b1r, b1i = left
        a2r, a2i, b2r, b2i = right
        return (a1r * a2r - a1i * a2i, a1r * a2i + a1i * a2r,
                a2r * b1r - a2i * b1i + b2r, a2r * b1i + a2i * b1r + b2i)

    _, _, xr, xi = lax.associative_scan(combine, (la_re, la_im, bu_re, bu_im), axis=1)
    y = (jnp.einsum('bsgn,gcn->bsgc', xr, c_re.astype(f32))
         - jnp.einsum('bsgn,gcn->bsgc', xi, c_im.astype(f32)))
    y = y.reshape(bsz, seq, dm) + d_skip.astype(f32) * x.astype(f32)
    h = jax.nn.gelu(y).astype(x.dtype) @ w_glu
    val, gate = jnp.split(h, 2, axis=-1)
    return (val * jax.nn.sigmoid(gate)).astype(x.dtype)


def memory_cross_attention(x, mem_k, mem_v, w_q, w_o):
    bsz, seq, dm = x.shape
    q = (x @ w_q).reshape(bsz, seq, M_HEADS, M_HEAD_DIM)
    s = jnp.einsum('bshd,bmhd->bhsm', q, mem_k).astype(jnp.float32) * (M_HEAD_DIM ** -0.5)
    p = jax.nn.softmax(s, axis=-1)
    o = jnp.einsum('bhsm,bmhd->bshd', p, mem_v.astype(jnp.float32))
    return o.reshape(bsz, seq, dm).astype(x.dtype) @ w_o


def clamped_swiglu(h):
    h_glu, h_lin = jnp.split(h, 2, axis=-1)
    h_glu = jnp.minimum(h_glu, SWIGLU_LIMIT)
    h_lin = jnp.clip(h_lin, -SWIGLU_LIMIT, SWIGLU_LIMIT)
    return h_glu * jax.nn.sigmoid(SWIGLU_ALPHA * h_glu) * (h_lin + 1.0)


def moe_ffn(x, w_r, b_r, w1, b1, w2, b2):
    bsz, seq, dm = x.shape
    n_tok = bsz * seq
    n_assign = n_tok * TOP_K
    xt = x.reshape(n_tok, dm)
    logits = (xt @ w_r + b_r).astype(jnp.float32)
    top_val, top_idx = lax.top_k(logits, TOP_K)
    gates = jax.nn.softmax(top_val, axis=-1)
    flat_e = top_idx.reshape(-1)
    order = jnp.argsort(flat_e)
    sorted_e = flat_e[order]
    tok = order // TOP_K
    counts = jnp.bincount(flat_e, length=N_EXPERTS)
    padded = (counts + MOE_BLOCK - 1) // MOE_BLOCK * MOE_BLOCK
    pad_end = jnp.cumsum(padded)
    slot = (pad_end - padded)[sorted_e] + (jnp.arange(n_assign) - (jnp.cumsum(counts) - counts)[sorted_e])
    n_blocks = -(-n_assign // MOE_BLOCK) + N_EXPERTS
    xs = jnp.zeros((n_blocks * MOE_BLOCK, dm), x.dtype).at[slot].set(xt[tok])
    block_e = jnp.minimum(jnp.searchsorted(pad_end, jnp.arange(n_blocks) * MOE_BLOCK, side='right'),
                          N_EXPERTS - 1)

    def expert_block(args):
        xb, e = args
        return clamped_swiglu(xb @ w1[e] + b1[e]) @ w2[e] + b2[e]

    ys = lax.map(expert_block, (xs.reshape(n_blocks, MOE_BLOCK, dm), block_e)).reshape(-1, dm)[slot]
    wts = gates.reshape(-1)[order][:, None].astype(ys.dtype)
    y = jnp.zeros_like(xt).at[tok].add(ys * wts)
    return y.reshape(bsz, seq, dm)


def setup_inputs(seed: int = 0) -> dict:
    key = jax.random.key(seed)
    ks = jax.random.split(key, 32)
    f32 = jnp.float32

    def nrm(k, shape, scale):
        return jax.random.normal(k, shape, f32) * scale

    d = D_MODEL
    s_in = d ** -0.5
    s_out = s_in * DEEPNORM_BETA
    positions = (jax.random.randint(ks[2], (BATCH, 1), 0, MAX_POS_OFFSET, dtype=jnp.int32)
                 + jnp.arange(SEQ, dtype=jnp.int32)[None, :])
    return {
        'x': nrm(ks[0], (BATCH, SEQ, d), 1.0),
        'mem': nrm(ks[1], (BATCH, MEM_LEN, d), 1.0),
        'positions': positions,
        'ln_g': 1.0 + nrm(ks[3], (DEPTH, 3, d), 0.02),
        'ln_b': nrm(ks[4], (DEPTH, 3, d), 0.02),
        'a_w_in': nrm(ks[5], (N_A, d, 4 * d), s_in),
        'a_lower_bounds': nrm(ks[6], (DEPTH, d), 0.1),
        'a_norm_g': 1.0 + nrm(ks[7], (N_A, d), 0.02),
        'a_w_out': nrm(ks[8], (N_A, d, d), s_out),
        'b_w_in': nrm(ks[9], (N_B, d, 3 * len(B_GROUPS) * d), s_in),
        'b_w_out': nrm(ks[10], (N_B, d, d), s_out),
        'c_a_re': -0.5 + nrm(ks[11], (N_C, C_GROUPS, C_STATE), 0.01),
        'c_a_im': math.pi * jnp.arange(C_STATE, dtype=f32) + nrm(ks[12], (N_C, C_GROUPS, C_STATE), 0.01),
        'c_log_dt': jax.random.uniform(ks[13], (N_C, C_GROUPS), f32, math.log(DT_MIN), math.log(DT_MAX)),
        'c_b_re': nrm(ks[14], (N_C, C_GROUPS, C_STATE, C_GROUP_CH), (2 * C_GROUP_CH) ** -0.5),
        'c_b_im': nrm(ks[15], (N_C, C_GROUPS, C_STATE, C_GROUP_CH), (2 * C_GROUP_CH) ** -0.5),
        'c_c_re': nrm(ks[16], (N_C, C_GROUPS, C_GROUP_CH, C_STATE), C_STATE ** -0.5),
        'c_c_im': nrm(ks[17], (N_C, C_GROUPS, C_GROUP_CH, C_STATE), C_STATE ** -0.5),
        'c_d': nrm(ks[18], (N_C, d), 0.5),
        'c_w_glu': jnp.concatenate([nrm(ks[19], (N_C, d, d), s_out), nrm(ks[20], (N_C, d, d), s_in)], axis=-1),
        'm_w_kv': nrm(ks[21], (d, 2 * d), s_in),
        'm_w_q': nrm(ks[22], (DEPTH, d, d), s_in),
        'm_w_o': nrm(ks[23], (DEPTH, d, d), s_out),
        'r_w': nrm(ks[24], (DEPTH, d, N_EXPERTS), s_in),
        'r_b': nrm(ks[25], (DEPTH, N_EXPERTS), 0.01),
        'e_w1': nrm(ks[26], (DEPTH, N_EXPERTS, d, 2 * D_EXPERT), s_in),
        'e_b1': nrm(ks[27], (DEPTH, N_EXPERTS, 2 * D_EXPERT), 0.01),
        'e_w2': nrm(ks[28], (DEPTH, N_EXPERTS, D_EXPERT, d), D_EXPERT ** -0.5 * DEEPNORM_BETA),
        'e_b2': nrm(ks[29], (DEPTH, N_EXPERTS, d), 0.01),
    }


def reference(x, mem, positions, ln_g, ln_b, a_w_in, a_lower_bounds, a_norm_g, a_w_out,
              b_w_in, b_w_out, c_a_re, c_a_im, c_log_dt, c_b_re, c_b_im, c_c_re, c_c_im,
              c_d, c_w_glu, m_w_kv, m_w_q, m_w_o, r_w, r_b, e_w1, e_b1, e_w2, e_b2):
    bsz, seq, dm = x.shape
    lb = jax.nn.softmax(a_lower_bounds.astype(jnp.float32), axis=0)
    lb = jnp.cumsum(lb, axis=0) - lb[0]
    mem_kv = (mem @ m_w_kv).reshape(bsz, mem.shape[1], 2, M_HEADS, M_HEAD_DIM)
    mem_k, mem_v = mem_kv[:, :, 0], mem_kv[:, :, 1]
    h = x
    for layer in range(DEPTH):
        kind, j = layer % N_MIXERS, layer // N_MIXERS
        if kind == 0:
            mix = hgrn2_mixer(h, a_w_in[j], a_norm_g[j], a_w_out[j], lb[layer])
        elif kind == 1:
            mix = dilated_mixer(h, positions, b_w_in[j], b_w_out[j])
        else:
            mix = s5_mixer(h, c_a_re[j], c_a_im[j], c_log_dt[j], c_b_re[j], c_b_im[j],
                           c_c_re[j], c_c_im[j], c_d[j], c_w_glu[j])
        h = layer_norm(DEEPNORM_ALPHA * h + mix, ln_g[layer, 0], ln_b[layer, 0])
        h = layer_norm(DEEPNORM_ALPHA * h + memory_cross_attention(h, mem_k, mem_v, m_w_q[layer], m_w_o[layer]),
                       ln_g[layer, 1], ln_b[layer, 1])
        h = layer_norm(DEEPNORM_ALPHA * h + moe_ffn(h, r_w[layer], r_b[layer], e_w1[layer], e_b1[layer],
                                                    e_w2[layer], e_b2[layer]),
                       ln_g[layer, 2], ln_b[layer, 2])
    return h
```

```python
import math
import numpy as np
import concourse.bass as bass
import concourse.mybir as mybir

F32 = mybir.dt.float32
BF16 = mybir.dt.bfloat16
ALU = mybir.AluOpType
AF = mybir.ActivationFunctionType
AX = mybir.AxisListType
ENGS = ("tensor", "vector", "scalar", "gpsimd", "sync")


class Ctr:
    def __init__(self, nc, name):
        self.name = name
        self.sem = nc.alloc_semaphore(name=name)
        self.count = 0

    def inc(self, n=1):
        self.count += n

    def value(self):
        return self.count


class MK:
    def __init__(self, nc):
        self.nc = nc
        self._keep = []
        self.cnt = {e: Ctr(nc, "p_" + e) for e in ("tensor", "vector", "scalar", "gpsimd")}
        self.dctr = {}
        self.lastw, self.readers, self.seen = {}, {}, {}
        self.seq = 0
        self.prev = {}

    def sb(self, name, shape, dt):
        cm = self.nc.sbuf_tensor(name, shape, dt)
        t = cm.__enter__()
        self._keep.append(cm)
        return t

    def ps(self, name, shape, dt=F32):
        cm = self.nc.psum_tensor(name, shape, dt)
        t = cm.__enter__()
        self._keep.append(cm)
        return t

    def dma_ctr(self, name):
        if name not in self.dctr:
            self.dctr[name] = Ctr(self.nc, "d_" + name)
        return self.dctr[name]

    def all_ctrs(self):
        return list(self.cnt.values()) + list(self.dctr.values())

    def _waits(self, eng, reads, writes):
        toks = []
        for k in reads:
            if k in self.lastw:
                toks.append(self.lastw[k])
        for k in writes:
            if k in self.lastw:
                toks.append(self.lastw[k])
            toks.extend(self.readers.get(k, []))
        if eng in ("vector", "scalar", "gpsimd") and eng in self.prev:
            toks.append(self.prev[eng])
        best = {}
        for (ctr, val, sq) in toks:
            if ctr.name not in best or best[ctr.name][2] < sq:
                best[ctr.name] = (ctr, val, sq)
        e = getattr(self.nc, eng)
        for name, (ctr, val, sq) in best.items():
            if self.seen.get((eng, name), -1) >= sq:
                continue
            e.wait_ge(ctr.sem, val)
            self.seen[(eng, name)] = sq

    def _done(self, eng, ins, reads, writes, dma):
        if dma is not None:
            ctr = self.dma_ctr(dma)
            ins.then_inc(ctr.sem, 16)
            ctr.inc(16)
        else:
            ctr = self.cnt[eng]
            ins.then_inc(ctr.sem, 1)
            ctr.inc(1)
        self.seq += 1
        tok = (ctr, ctr.value(), self.seq)
        if dma is None:
            self.prev[eng] = tok
        for k in writes:
            self.lastw[k] = tok
            self.readers[k] = []
        for k in reads:
            self.readers.setdefault(k, []).append(tok)
        return tok

    def op(self, eng, fn, reads=(), writes=(), dma=None):
        self._waits(eng, reads, writes)
        ins = fn(getattr(self.nc, eng))
        return self._done(eng, ins, reads, writes, dma)

    def dma(self, eng, fns, reads=(), writes=(), ctr="misc"):
        self._waits(eng, reads, writes)
        c = self.dma_ctr(ctr)
        if c.count > 0:
            getattr(self.nc, eng).wait_ge(c.sem, c.value())
        for fn in fns:
            fn(getattr(self.nc, eng)).then_inc(c.sem, 16)
            c.inc(16)
        self.seq += 1
        tok = (c, c.value(), self.seq)
        for k in writes:
            self.lastw[k] = tok
            self.readers[k] = []
        for k in reads:
            self.readers.setdefault(k, []).append(tok)
        return tok

    def pe(self, fns, reads=(), writes=()):
        self._waits("tensor", reads, writes)
        ins = None
        for fn in fns:
            ins = fn(self.nc.tensor)
        return self._done("tensor", ins, reads, writes, None)

    def sync_all(self):
        for eng in ENGS:
            e = getattr(self.nc, eng)
            for ctr in self.all_ctrs():
                e.wait_ge(ctr.sem, ctr.value())
        self.nc.all_engine_barrier()
        self.lastw, self.readers, self.seen, self.prev = {}, {}, {}, {}

    def reset_all(self):
        nc = self.nc
        self.sync_all()
        for eng in ENGS:
            getattr(nc, eng).drain()
        nc.all_engine_barrier()
        for ctr in self.all_ctrs():
            if ctr.name.startswith("d_sw"):
                continue
            nc.sync.sem_clear(ctr.sem)
            ctr.count = 0
        nc.all_engine_barrier()


def setup_consts(mk, E, D):
    mk.ident = mk.sb("ident", [128, 128], F32)
    mk.onesD = mk.sb("onesD", [128, 128], F32)
    mk.ones1 = mk.sb("ones1", [1, 128], F32)
    mk.onesE = mk.sb("onesE", [E, 128], F32)
    mk.selb = [mk.sb(f"selb{i}", [E, 128], F32) for i in range(2)]
    mk.op("gpsimd", lambda g: g.memset(mk.ident[:], 1.0), writes=["ident"])
    mk.op("gpsimd", lambda g: g.memset(mk.onesE[:], 1.0), writes=["onesE"])
    mk.op("vector", lambda v: v.memset(mk.onesD[:], 1.0 / D), writes=["onesD"])
    mk.op("vector", lambda v: v.memset(mk.ones1[:], 1.0), writes=["ones1"])
    mk.ones_bf = mk.sb("ones_bf", [128, 128], BF16)
    mk.op("vector", lambda v: v.memset(mk.ones_bf[:], 1.0), writes=["ones_bf"])
    mk.op("gpsimd", lambda g: g.affine_select(out=mk.ident[:], in_=mk.ident[:], pattern=[[-1, 128]],
                                              compare_op=ALU.is_equal, fill=0.0, base=0, channel_multiplier=1),
          reads=["ident"], writes=["ident"])


def load_rows_T(mk, rows_ap, R, L, out_sb, out_key, tmp_sb, ps, ps_key):
    mk.dma("sync", [lambda s: s.dma_start(out=tmp_sb[0:R, 0:L], in_=rows_ap)], writes=["tmprows"], ctr="tmprows")
    for c in range(L // 128):
        mk.pe([lambda t, c=c: t.transpose(out=ps[0:128, 0:R], in_=tmp_sb[0:R, c * 128:(c + 1) * 128], identity=mk.ident[0:R, 0:R])],
              reads=["tmprows", "ident"], writes=[ps_key])
        mk.op("vector", lambda v, c=c: v.tensor_copy(out=out_sb[:, c, 0:R], in_=ps[0:128, 0:R]), reads=[ps_key], writes=[out_key])


def ln_group(mk, z, zk, C, n, gT, bT, li, out_f, outk, ps_mean, pmk, ps_ex2, pek, sq, mean_sb, rstd_sb, eps=1e-5, sqkey="w1sb1"):
    mk.op("scalar", lambda a: a.activation(out=sq, in_=z[:], func=AF.Square), reads=[zk], writes=["sq", sqkey])
    mk.pe([lambda t, c=c: t.matmul(ps_mean, lhsT=mk.onesD[:], rhs=z[:, c, :], start=(c == 0), stop=(c == C - 1)) for c in range(C)],
          reads=[zk, "onesD"], writes=[pmk])
    mk.pe([lambda t, c=c: t.matmul(ps_ex2, lhsT=mk.onesD[:], rhs=sq[:, c, :], start=(c == 0), stop=(c == C - 1)) for c in range(C)],
          reads=["sq", sqkey, "onesD"], writes=[pek])
    mk.op("vector", lambda v: v.tensor_copy(out=mean_sb[:], in_=ps_mean), reads=[pmk], writes=["mean"])
    mk.op("vector", lambda v: v.tensor_tensor(out=rstd_sb[:], in0=ps_mean, in1=mean_sb[:], op=ALU.mult), reads=[pmk, "mean"], writes=["rstd"])
    mk.op("vector", lambda v: v.tensor_tensor(out=rstd_sb[:], in0=ps_ex2, in1=rstd_sb[:], op=ALU.subtract), reads=[pek, "rstd"], writes=["rstd"])
    mk.op("vector", lambda v: v.tensor_scalar(out=rstd_sb[:], in0=rstd_sb[:], scalar1=eps, scalar2=None, op0=ALU.add), reads=["rstd"], writes=["rstd"])
    mk.op("scalar", lambda a: a.activation(out=rstd_sb[:], in_=rstd_sb[:], func=AF.Sqrt), reads=["rstd"], writes=["rstd"])
    mk.op("vector", lambda v: v.reciprocal(out=rstd_sb[:], in_=rstd_sb[:]), reads=["rstd"], writes=["rstd"])
    for c in range(C):
        mk.op("vector", lambda v, c=c: v.tensor_tensor(out=z[:, c, :], in0=z[:, c, :], in1=mean_sb[:], op=ALU.subtract), reads=[zk, "mean"], writes=[zk])
        mk.op("vector", lambda v, c=c: v.tensor_tensor(out=z[:, c, :], in0=z[:, c, :], in1=rstd_sb[:], op=ALU.mult), reads=[zk, "rstd"], writes=[zk])
        mk.op("vector", lambda v, c=c: v.tensor_scalar(out=out_f[:, c, :], in0=z[:, c, :], scalar1=gT[:, c, li:li + 1],
                                                       scalar2=bT[:, c, li:li + 1], op0=ALU.mult, op1=ALU.add),
              reads=[zk, "gT", "bT"], writes=[outk])


def moe_alloc(mk, C, Fc, E, n=512):
    k = mk
    S = {}
    NS = n // 128
    S["hf"] = k.sb("hf", [128, C, n], F32)
    S["hb"] = k.sb("hb", [128, C, n], BF16)
    S["yacc"] = k.sb("yacc", [128, C, n], F32)
    S["aT"] = k.sb("aT", [128, Fc, n], BF16)
    S["w1sb"] = [k.sb(f"w1sb{i}", [128, C, 2 * Fc * 128], BF16) for i in range(2)]
    S["w2sb"] = [k.sb(f"w2sb{i}", [128, Fc, C * 128], BF16) for i in range(2)]
    for nm in ("gsb", "ssb", "lsb"):
        S[nm] = [k.sb(f"{nm}{i}", [128, n], F32) for i in range(2)]
    S["usb"] = k.sb("usb", [128, n], F32)
    S["mean_sb"] = k.sb("mean_sb", [128, n], F32)
    S["rstd_sb"] = k.sb("rstd_sb", [128, n], F32)
    S["lg"] = k.sb("lg", [128, NS * E], F32)
    S["mx8"] = k.sb("mx8", [128, NS, 8], F32)
    S["negmx"] = k.sb("negmx", [128, NS], F32)
    S["msk"] = k.sb("msk", [128, NS * E], F32)
    S["ex"] = k.sb("ex", [128, NS * E], F32)
    S["ssum"] = k.sb("ssum", [128, NS], F32)
    S["gates"] = k.sb("gates", [128, NS * E], F32)
    S["gatesT"] = k.sb("gatesT", [E, n], F32)
    S["psA"] = [k.ps(f"psA{i}", [128, 2, n]) for i in range(2)]
    S["psG"] = k.ps("psG", [128, n])
    S["psY"] = [k.ps(f"psY{i}", [128, n]) for i in range(2)]
    S["psM"] = k.ps("psM", [128, n])
    if Fc * 128 >= n:
        S["sq"] = S["w1sb"][1][:].bitcast(F32)[:, :, 0:n]
    else:
        S["sq"] = k.sb("sq", [128, C, n], F32)[:]
    for nm in ("ld", "st", "w0", "w1", "tmprows"):
        k.dma_ctr(nm)
    return S


def moe_phase(mk, S, hsrc, hdst, w1d, w2d, b1T, b2sb, rw, rb, gT, bT, li, alpha, C, Fc, E, T, n=512, limit=7.0):
    nc = mk.nc
    NG = T // n
    NS = n // 128
    hf, hb, yacc, aT = S["hf"], S["hb"], S["yacc"], S["aT"]
    w1sb, w2sb = S["w1sb"], S["w2sb"]
    gsb, ssb, lsb, usb = S["gsb"], S["ssb"], S["lsb"], S["usb"]
    psA, psG, psY, psM = S["psA"], S["psG"], S["psY"], S["psM"]
    lg, mx8, negmx, msk, ex, ssum, gates, gatesT = (S[x] for x in ("lg", "mx8", "negmx", "msk", "ex", "ssum", "gates", "gatesT"))

    def load_w(e):
        b = e % 2
        j1, j2 = w1d[1], w2d[1]
        mk.dma("sync", [lambda s, q=q: s.dma_start(out=w1sb[b][:, q * j1:(q + 1) * j1, :], in_=w1d[0](e)[:, q]) for q in range(C // j1)] +
                       [lambda s, q=q: s.dma_start(out=w2sb[b][:, q * j2:(q + 1) * j2, :], in_=w2d[0](e)[:, q]) for q in range(Fc // j2)],
               writes=[f"w1sb{b}", f"w2sb{b}"], ctr=f"w{b}")

    mk.reset_all()
    with nc.Fori(0, NG) as gi:
        load_w(0)
        mk.dma("sync", [lambda s: s.dma_start(out=hf[:], in_=hsrc[:, :, bass.ts(gi, n)].rearrange("c p t -> p c t"))], writes=["hf"], ctr="ld")
        mk.op("scalar", lambda a: a.copy(out=hb[:], in_=hf[:]), reads=["hf"], writes=["hb"])
        fns = []
        for s in range(NS):
            for c in range(C):
                fns.append(lambda t, s=s, c=c: t.matmul(psM[:, s * E:(s + 1) * E], lhsT=hf[:, c, s * 128:(s + 1) * 128], rhs=rw[:, c, :],
                                                        start=(c == 0), stop=False))
            fns.append(lambda t, s=s: t.matmul(psM[:, s * E:(s + 1) * E], lhsT=mk.ones1[0:1, :], rhs=rb[0:1, :], start=False, stop=True))
        mk.pe(fns, reads=["hf", "rw", "rb", "ones1"], writes=["psM"])
        V = lambda fn, r, w: mk.op("vector", fn, reads=r, writes=w)
        V(lambda v: v.tensor_copy(out=lg[:], in_=psM[:, 0:NS * E]), ["psM"], ["lg"])
        for s in range(NS):
            V(lambda v, s=s: v.max(out=mx8[:, s, :], in_=lg[:, s * E:(s + 1) * E]), ["lg"], ["mx8"])
        V(lambda v: v.tensor_scalar(out=negmx[:], in0=mx8[:, :, 0], scalar1=-1.0, scalar2=None, op0=ALU.mult), ["mx8"], ["negmx"])
        for s in range(NS):
            V(lambda v, s=s: v.tensor_scalar(out=msk[:, s * E:(s + 1) * E], in0=lg[:, s * E:(s + 1) * E],
                                             scalar1=mx8[:, s, 3:4], scalar2=None, op0=ALU.is_ge), ["lg", "mx8"], ["msk"])
        for s in range(NS):
            mk.op("scalar", lambda a, s=s: a.activation(out=ex[:, s * E:(s + 1) * E], in_=lg[:, s * E:(s + 1) * E], func=AF.Exp,
                                                        bias=negmx[:, s:s + 1], scale=1.0), reads=["lg", "negmx"], writes=["ex"])
        V(lambda v: v.tensor_tensor(out=ex[:], in0=ex[:], in1=msk[:], op=ALU.mult), ["ex", "msk"], ["ex"])
        V(lambda v: v.reduce_sum(out=ssum[:], in_=ex[:].rearrange("p (s e) -> p s e", e=E), axis=AX.X), ["ex"], ["ssum"])
        V(lambda v: v.reciprocal(out=ssum[:], in_=ssum[:]), ["ssum"], ["ssum"])
        for s in range(NS):
            V(lambda v, s=s: v.tensor_scalar(out=gates[:, s * E:(s + 1) * E], in0=ex[:, s * E:(s + 1) * E],
                                             scalar1=ssum[:, s:s + 1], scalar2=None, op0=ALU.mult), ["ex", "ssum"], ["gates"])
        mk.pe([lambda t, s=s: t.transpose(out=psG[0:E, s * 128:(s + 1) * 128], in_=gates[:, s * E:(s + 1) * E], identity=mk.ident[:])
               for s in range(NS)], reads=["gates", "ident"], writes=["psG"])
        V(lambda v: v.tensor_copy(out=gatesT[:], in_=psG[0:E, 0:n]), ["psG"], ["gatesT"])
        for m in range(C):
            mk.pe([lambda t, m=m: t.matmul(psY[m % 2][:], lhsT=b2sb[0:E, m * 128:(m + 1) * 128], rhs=gatesT[0:E, :], start=True, stop=True)],
                  reads=["b2sb", "gatesT"], writes=[f"psY{m % 2}"])
            V(lambda v, m=m: v.tensor_copy(out=yacc[:, m, :], in_=psY[m % 2][:]), [f"psY{m % 2}"], [f"yacc{m}"])
        for e in range(E):
            b = e % 2
            if e + 1 < E:
                load_w(e + 1)
            mk.op("gpsimd", lambda g: g.affine_select(out=mk.selb[b][:], in_=mk.onesE[:], pattern=[[0, 128]], compare_op=ALU.is_equal,
                                                      fill=0.0, base=-e, channel_multiplier=1), reads=["onesE"], writes=[f"selb{b}"])
            mk.pe([lambda t: t.matmul(psG[:], lhsT=mk.selb[b][:], rhs=gatesT[0:E, :], start=True, stop=True)],
                  reads=[f"selb{b}", "gatesT"], writes=["psG"])
            for j in range(Fc):
                jb = j % 2
                fns = []
                for half, fc in ((0, j), (1, Fc + j)):
                    for c in range(C):
                        fns.append(lambda t, half=half, fc=fc, c=c: t.matmul(psA[jb][:, half, :], lhsT=w1sb[b][:, c, fc * 128:(fc + 1) * 128],
                                                                             rhs=hb[:, c, :], start=(c == 0), stop=(c == C - 1)))
                mk.pe(fns, reads=[f"w1sb{b}", "hb"], writes=[f"psA{jb}"])
                mk.op("scalar", lambda a: a.activation(out=lsb[jb][:], in_=psA[jb][:, 1, :], func=AF.Identity,
                                                       bias=b1T[:, Fc + j, e:e + 1], scale=1.0), reads=[f"psA{jb}", "b1T"], writes=[f"lsb{jb}"])
                V(lambda v: v.tensor_scalar(out=gsb[jb][:], in0=psA[jb][:, 0, :], scalar1=b1T[:, j, e:e + 1], scalar2=limit,
                                            op0=ALU.add, op1=ALU.min), [f"psA{jb}", "b1T"], [f"gsb{jb}"])
                mk.op("scalar", lambda a: a.activation(out=ssb[jb][:], in_=gsb[jb][:], func=AF.Sigmoid, scale=1.702),
                      reads=[f"gsb{jb}"], writes=[f"ssb{jb}"])
                V(lambda v: v.tensor_scalar(out=lsb[jb][:], in0=lsb[jb][:], scalar1=-limit, scalar2=limit, op0=ALU.max, op1=ALU.min),
                  [f"lsb{jb}"], [f"lsb{jb}"])
                V(lambda v: v.tensor_tensor(out=usb[:], in0=gsb[jb][:], in1=ssb[jb][:], op=ALU.mult), [f"gsb{jb}", f"ssb{jb}"], ["usb"])
                V(lambda v: v.scalar_tensor_tensor(out=usb[:], in0=lsb[jb][:], scalar=1.0, in1=usb[:], op0=ALU.add, op1=ALU.mult),
                  [f"lsb{jb}", "usb"], ["usb"])
                V(lambda v: v.tensor_tensor(out=aT[:, j, :], in0=usb[:], in1=psG[:], op=ALU.mult), ["usb", "psG"], [f"aT{j}"])
            for m in range(C):
                mk.pe([lambda t, kk=kk: t.matmul(psY[m % 2][:], lhsT=w2sb[b][:, kk, m * 128:(m + 1) * 128], rhs=aT[:, kk, :],
                                                 start=(kk == 0), stop=(kk == Fc - 1)) for kk in range(Fc)],
                      reads=[f"w2sb{b}"] + [f"aT{kk}" for kk in range(Fc)], writes=[f"psY{m % 2}"])
                V(lambda v: v.tensor_tensor(out=yacc[:, m, :], in0=yacc[:, m, :], in1=psY[m % 2][:], op=ALU.add),
                  [f"psY{m % 2}", f"yacc{m}"], [f"yacc{m}"])
        yk = [f"yacc{m}" for m in range(C)]
        V(lambda v: v.scalar_tensor_tensor(out=yacc[:], in0=hf[:], scalar=float(alpha), in1=yacc[:], op0=ALU.mult, op1=ALU.add),
          ["hf"] + yk, yk + ["yaccall"])
        ln_group(mk, yacc, "yaccall", C, n, gT, bT, li, hf, "hf", psA[0][:, 0, :], "psA0", psA[0][:, 1, :], "psA0b",
                 S["sq"], S["mean_sb"], S["rstd_sb"])
        mk.dma("sync", [lambda s: s.dma_start(out=hdst[:, :, bass.ts(gi, n)].rearrange("c p t -> p c t"), in_=hf[:])], reads=["hf"], ctr="st")
        mk.reset_all()


D_MODEL, BATCH, SEQ, DEPTH = 1024, 16, 4096, 4
N_EXPERTS, D_EXPERT = 32, 1024
NCORES = 8
ALPHA = (2 * DEPTH) ** 0.25


GROUP = 4


def build_program(T=BATCH * SEQ // NCORES, D=D_MODEL, F=D_EXPERT, E=N_EXPERTS, L=DEPTH, ncores=NCORES, gs=GROUP, NSEQ=2, M=256):
    C, Fc = D // 128, F // 128
    SEQL = T // NSEQ
    XA = (D == 1024 and NSEQ * M == 512)
    EL = E // gs
    nc = bass.Bass("TRN2", target_bir_lowering=False)
    xT = nc.dram_tensor("xT", [C, 128, T], F32, kind="ExternalInput").ap()
    w1s = nc.dram_tensor("w1s", [L * EL * D, 2 * F], F32, kind="ExternalInput").ap()
    w2s = nc.dram_tensor("w2s", [L * EL * F, D], F32, kind="ExternalInput").ap()
    b1 = nc.dram_tensor("b1", [L, E, 2 * F], F32, kind="ExternalInput").ap()
    b2 = nc.dram_tensor("b2", [L, E, D], F32, kind="ExternalInput").ap()
    rwd = nc.dram_tensor("rw", [L, D, E], F32, kind="ExternalInput").ap()
    rbd = nc.dram_tensor("rb", [L, 1, E], F32, kind="ExternalInput").ap()
    lng = nc.dram_tensor("lng", [L, 3, D], F32, kind="ExternalInput").ap()
    lnb = nc.dram_tensor("lnb", [L, 3, D], F32, kind="ExternalInput").ap()
    oT = nc.dram_tensor("oT", [C, 128, T], F32, kind="ExternalOutput").ap()
    HG = XA
    if HG:
        NA = (L + 2) // 3
        awin = nc.dram_tensor("awin", [NA, D, 4 * D], F32, kind="ExternalInput").ap()
        awout = nc.dram_tensor("awout", [NA, D, D], F32, kind="ExternalInput").ap()
        alb = nc.dram_tensor("alb", [L, D], F32, kind="ExternalInput").ap()
        ang = nc.dram_tensor("ang", [NA, 1, D], F32, kind="ExternalInput").ap()
    DL = XA and L >= 2
    if DL:
        NBL = (L + 1) // 3
        bwin = nc.dram_tensor("bwin", [NBL, D, 9 * D], F32, kind="ExternalInput").ap()
        bwout = nc.dram_tensor("bwout", [NBL, D, D], F32, kind="ExternalInput").ap()
        posd = nc.dram_tensor("posd", [NSEQ, 1, SEQL], mybir.dt.int32, kind="ExternalInput").ap()
        attn_d = nc.dram_tensor("attn_d", [C, 128, T], BF16).ap()
    S5 = XA and L >= 3
    if S5:
        NCL = (L + 0) // 3
        s5prm = nc.dram_tensor("s5prm", [NCL, 3, 32, 128], F32, kind="ExternalInput").ap()
        s5pad = nc.dram_tensor("s5pad", [NCL, 32, 4, 128, 128], F32, kind="ExternalInput").ap()
        s5wglu = nc.dram_tensor("s5wglu", [NCL, D, 2 * D], F32, kind="ExternalInput").ap()
        s5cd = nc.dram_tensor("s5cd", [NCL, 1, D], F32, kind="ExternalInput").ap()
    if XA:
        memT = nc.dram_tensor("memT", [NSEQ, C, 128, M], F32, kind="ExternalInput").ap()
        wkv = nc.dram_tensor("wkv", [D, 2 * D], F32, kind="ExternalInput").ap()
        wqd = nc.dram_tensor("wq", [L, D, D], F32, kind="ExternalInput").ap()
        wod = nc.dram_tensor("wo", [L, D, D], F32, kind="ExternalInput").ap()
    hbuf = [nc.dram_tensor(f"hbuf{i}", [C, 128, T], F32).ap() for i in range(3)]
    w1c = nc.dram_tensor("w1c", [L * EL * D, 2 * F], BF16)
    w2c = nc.dram_tensor("w2c", [L * EL * F, D], BF16)
    CH1 = min(256, D)
    CH2 = min(512, F)
    w1g = [nc.dram_tensor(f"w1g{l}", [gs * EL * D, 2 * F], BF16) for l in range(L)]
    w2g = [nc.dram_tensor(f"w2g{l}", [gs * EL * F, D], BF16) for l in range(L)]

    mk = MK(nc)
    for nm in ("ld", "st", "w0", "w1", "tmprows", "sw", "params", "misc"):
        mk.dma_ctr(nm)
    for ctr in mk.all_ctrs():
        nc.sync.sem_clear(ctr.sem)
    nc.all_engine_barrier()
    setup_consts(mk, E, D)
    S = moe_alloc(mk, C, Fc, E)
    if HG:
        hgrn2_consts(mk)
        H = hgrn2_alloc(mk, C, L)
    RB = min(1024, EL * D, EL * F)
    groups = [list(range(g0, g0 + gs)) for g0 in range(0, ncores, gs)]
    fns = [lambda g, r=r: g.dma_start(out=w1c.ap()[r:r + RB, :], in_=w1s[r:r + RB, :]) for r in range(0, L * EL * D, RB)]
    fns += [lambda g, r=r: g.dma_start(out=w2c.ap()[r:r + RB, :], in_=w2s[r:r + RB, :]) for r in range(0, L * EL * F, RB)]
    import os
    NB = int(os.environ.get("MK_NB", "8"))
    for i in range(0, len(fns), NB):
        if i:
            nc.gpsimd.drain()
        mk.dma("gpsimd", fns[i:i + NB], writes=["wc"], ctr="sw")
    ncc = 0
    for l in range(L):
        for (src, dstt, rows, CH, key) in ((w1c, w1g[l], EL * D, CH1, f"w1g{l}"), (w2c, w2g[l], EL * F, CH2, f"w2g{l}")):
            for i in range(rows // CH):
                mk.op("gpsimd", lambda g, src=src, dstt=dstt, rows=rows, CH=CH, i=i, l=l: g.collective_compute(
                    "AllGather", ALU.bypass, replica_groups=groups,
                    ins=[src.ap()[l * rows + i * CH:l * rows + (i + 1) * CH, :]],
                    outs=[dstt.ap()[i * gs * CH:(i + 1) * gs * CH, :]]), reads=["wc"], writes=[key])
                ncc += 1
    b1T = mk.sb("b1T", [128, 2 * Fc, E], F32)
    b2sb = mk.sb("b2sb", [E, D], F32)
    rw = mk.sb("rwsb", [128, C, E], F32)
    rb = mk.sb("rbsb", [1, E], F32)
    gT = mk.sb("gT", [128, C, 3], F32)
    bT = mk.sb("bT", [128, C, 3], F32)
    if C * 512 >= 2 * F:
        tmp = S["yacc"][:].rearrange("p c n -> p (c n)")
    else:
        tmp = mk.sb("tmprows", [E, 2 * F], F32)
    if XA:
        kT_sb = mk.sb("kT_sb", [128, C, NSEQ * M], BF16)
        v_sb = mk.sb("v_sb", [128, NSEQ * M // 128, D], BF16)
        xattn_setup(mk, S, memT, wkv, kT_sb, v_sb, C, NSEQ, M)
    for l in range(L):
        load_rows_T(mk, b1[l], E, 2 * F, b1T, "b1T", tmp, S["psM"], "psM")
        load_rows_T(mk, lng[l], 3, D, gT, "gT", tmp, S["psM"], "psM")
        load_rows_T(mk, lnb[l], 3, D, bT, "bT", tmp, S["psM"], "psM")
        mk.dma("sync", [lambda s_: s_.dma_start(out=b2sb[:], in_=b2[l]),
                        lambda s_: s_.dma_start(out=rw[:], in_=rwd[l].rearrange("(c p) e -> p c e", p=128)),
                        lambda s_: s_.dma_start(out=rb[:], in_=rbd[l])], writes=["b2sb", "rw", "rb"], ctr="params")
        src = xT if l == 0 else hbuf[1]
        dst = oT if l == L - 1 else hbuf[1]
        if HG and l % 3 == 0:
            hgrn2_phase(mk, S, H, src, hbuf[2], awin[l // 3], awout[l // 3], alb, ang[l // 3], l, L, tmp, gT, bT, 0, ALPHA, C, NSEQ, SEQL)
            src = hbuf[2]
        if DL and l % 3 == 1:
            dil_phase(mk, S, src, hbuf[2], attn_d, bwin[l // 3], bwout[l // 3], posd, gT, bT, 0, ALPHA, C, NSEQ, SEQL)
            src = hbuf[2]
        if S5 and l % 3 == 2:
            s5_phase(mk, S, src, hbuf[2], s5prm[l // 3], s5pad[l // 3], s5wglu[l // 3], s5cd[l // 3], tmp, gT, bT, 0, ALPHA, C, NSEQ, SEQL)
            src = hbuf[2]
        if XA:
            xattn_phase(mk, S, src, hbuf[0], wqd[l], wod[l], kT_sb, v_sb, mk.ones_bf, gT, bT, 1, ALPHA, C, NSEQ, SEQL, M)
            src = hbuf[0]
        w1v5 = w1g[l].ap().rearrange("(el dq r jh p) f -> r el p dq jh f", el=EL, r=gs, jh=CH1 // 128, p=128)
        w2v5 = w2g[l].ap().rearrange("(el dq r jh p) f -> r el p dq jh f", el=EL, r=gs, jh=CH2 // 128, p=128)
        w1v = (lambda e, v=w1v5: v[e // EL, e % EL], CH1 // 128)
        w2v = (lambda e, v=w2v5: v[e // EL, e % EL], CH2 // 128)
        moe_phase(mk, S, src, dst, w1v, w2v, b1T, b2sb, rw, rb, gT, bT, 2, ALPHA, C, Fc, E, T)
    mk.sync_all()
    return nc


_NC_CACHE = {}


def kernel(x, mem, positions, ln_g, ln_b, a_w_in, a_lower_bounds, a_norm_g, a_w_out, b_w_in, b_w_out,
           c_a_re, c_a_im, c_log_dt, c_b_re, c_b_im, c_c_re, c_c_im, c_d, c_w_glu, m_w_kv, m_w_q, m_w_o,
           r_w, r_b, e_w1, e_b1, e_w2, e_b2):
    from concourse.bass_utils import run_bass_kernel_spmd
    x = np.asarray(x, np.float32)
    B, Sq, D = x.shape
    T = B * Sq // NCORES
    C = D // 128
    L, E = e_w1.shape[0], e_w1.shape[1]
    EL = E // GROUP
    if "nc" not in _NC_CACHE:
        _NC_CACHE["nc"] = build_program()
    nc = _NC_CACHE["nc"]
    e_w1 = np.asarray(e_w1, np.float32)
    e_w2 = np.asarray(e_w2, np.float32)
    common = dict(
        b1=np.ascontiguousarray(e_b1, np.float32), b2=np.ascontiguousarray(e_b2, np.float32),
        rw=np.ascontiguousarray(r_w, np.float32), rb=np.ascontiguousarray(np.asarray(r_b, np.float32)[:, None, :]),
        lng=np.ascontiguousarray(ln_g, np.float32), lnb=np.ascontiguousarray(ln_b, np.float32),
        wkv=np.ascontiguousarray(m_w_kv, np.float32), wq=np.ascontiguousarray(m_w_q, np.float32),
        wo=np.ascontiguousarray(m_w_o, np.float32),
        awin=np.ascontiguousarray(a_w_in, np.float32), awout=np.ascontiguousarray(a_w_out, np.float32),
        alb=np.ascontiguousarray(a_lower_bounds, np.float32),
        ang=np.ascontiguousarray(np.asarray(a_norm_g, np.float32)[:, None, :]),
        s5wglu=np.ascontiguousarray(c_w_glu, np.float32), s5cd=np.ascontiguousarray(np.asarray(c_d, np.float32)[:, None, :]))
    ncl = np.asarray(c_a_re).shape[0]
    s5prm = np.stack([np.stack([np.asarray(c_a_re[j], np.float32).reshape(32, 128), np.asarray(c_a_im[j], np.float32).reshape(32, 128),
                                np.repeat(np.asarray(c_log_dt[j], np.float32), 64).reshape(32, 128)]) for j in range(ncl)])
    s5pad = np.zeros((ncl, 32, 4, 128, 128), np.float32)
    for j in range(ncl):
        for tau in range(32):
            for g2 in range(2):
                g = 2 * tau + g2
                gl = g % 8
                s5pad[j, tau, 0, gl * 16:(gl + 1) * 16, g2 * 64:(g2 + 1) * 64] = np.asarray(c_b_re[j][g]).T
                s5pad[j, tau, 1, gl * 16:(gl + 1) * 16, g2 * 64:(g2 + 1) * 64] = np.asarray(c_b_im[j][g]).T
                s5pad[j, tau, 2, g2 * 64:(g2 + 1) * 64, gl * 16:(gl + 1) * 16] = np.asarray(c_c_re[j][g]).T
                s5pad[j, tau, 3, g2 * 64:(g2 + 1) * 64, gl * 16:(gl + 1) * 16] = np.asarray(c_c_im[j][g]).T
    common.update(s5prm=s5prm, s5pad=s5pad, bwin=np.ascontiguousarray(b_w_in, np.float32), bwout=np.ascontiguousarray(b_w_out, np.float32))
    pos_all = np.asarray(positions, np.int32).reshape(NCORES, 2, 1, Sq)
    in_maps = []
    for c in range(NCORES):
        xc = x.reshape(NCORES, T, D)[c]
        xTc = np.ascontiguousarray(xc.T).reshape(C, 128, T)
        r = c % GROUP
        w1c = np.ascontiguousarray(e_w1[:, r * EL:(r + 1) * EL]).reshape(L * EL * D, -1)
        w2c = np.ascontiguousarray(e_w2[:, r * EL:(r + 1) * EL]).reshape(L * EL * e_w2.shape[2], -1)
        memc = np.asarray(mem, np.float32).reshape(NCORES, 2, mem.shape[1], D)[c]
        memT = np.ascontiguousarray(memc.transpose(0, 2, 1)).reshape(2, C, 128, mem.shape[1])
        in_maps.append(dict(xT=xTc, w1s=w1c, w2s=w2c, memT=memT, posd=np.ascontiguousarray(pos_all[c]), **common))
    res = run_bass_kernel_spmd(nc, in_maps, core_ids=list(range(NCORES)))
    out = np.empty((NCORES, T, D), np.float32)
    for c in range(NCORES):
        out[c] = res.results[c]["oT"].reshape(D, T).T
    return out.reshape(B, Sq, D)


def xattn_setup(mk, S, memT, wkv, kT_sb, v_sb, C, NSEQ, M=256):
    hf, hb, psY = S["hf"], S["hb"], S["psY"]
    wsb = S["w1sb"][0]
    D = C * 128
    mk.dma("gpsimd", [lambda g: g.dma_start(out=wsb[:], in_=wkv.rearrange("(c p) f -> p c f", p=128))], writes=["w1sb0"], ctr="sw")
    mk.dma("sync", [lambda s_, s=s: s_.dma_start(out=hf[:, :, s * M:(s + 1) * M], in_=memT[s].rearrange("c p m -> p c m")) for s in range(NSEQ)],
           writes=["hf"], ctr="ld")
    mk.op("scalar", lambda a: a.copy(out=hb[:, :, 0:NSEQ * M], in_=hf[:, :, 0:NSEQ * M]), reads=["hf"], writes=["hb"])
    for j in range(C):
        mk.pe([lambda t, c=c: t.matmul(psY[j % 2][:, 0:NSEQ * M], lhsT=wsb[:, c, j * 128:(j + 1) * 128], rhs=hb[:, c, 0:NSEQ * M],
                                       start=(c == 0), stop=(c == C - 1)) for c in range(C)], reads=["w1sb0", "hb"], writes=[f"psY{j % 2}"])
        mk.op("scalar", lambda a: a.copy(out=kT_sb[:, j, :], in_=psY[j % 2][:, 0:NSEQ * M]), reads=[f"psY{j % 2}"], writes=["kT"])
    i = 0
    for mt in range(NSEQ * M // 128):
        for half in range(D // 512):
            mk.pe([lambda t, c=c: t.matmul(psY[i % 2][:], lhsT=hb[:, c, mt * 128:(mt + 1) * 128], rhs=wsb[:, c, D + half * 512:D + (half + 1) * 512],
                                           start=(c == 0), stop=(c == C - 1)) for c in range(C)], reads=["w1sb0", "hb"], writes=[f"psY{i % 2}"])
            mk.op("vector", lambda v: v.tensor_copy(out=v_sb[:, mt, half * 512:(half + 1) * 512], in_=psY[i % 2][:]),
                  reads=[f"psY{i % 2}"], writes=["vsb"])
            i += 1


def xattn_phase(mk, S, hsrc, hdst, wq, wo, kT_sb, v_sb, ones_bf, gT, bT, li, alpha, C, NSEQ, SEQ, M=256, NH=4, n=512):
    nc = mk.nc
    hf, hb, yacc, aT, usb = S["hf"], S["hb"], S["yacc"], S["aT"], S["usb"]
    psA, psG, psY = S["psA"], S["psG"], S["psY"]
    wqsb, wosb = S["w2sb"][0], S["w2sb"][1]
    expT = [S["gsb"][i][:].bitcast(BF16)[:, 0:n] for i in range(2)]
    DH = C * 128 // NH
    dcs = DH // 128
    scale = float(DH) ** -0.5
    V = lambda fn, r, w: mk.op("vector", fn, reads=r, writes=w)
    mk.dma("gpsimd", [lambda g: g.dma_start(out=wqsb[:], in_=wq.rearrange("(c p) f -> p c f", p=128)),
                      lambda g: g.dma_start(out=wosb[:], in_=wo.rearrange("(c p) f -> p c f", p=128))], writes=["w2sb0", "w2sb1"], ctr="sw")
    mk.reset_all()
    for s in range(NSEQ):
        for gi in range(SEQ // n):
            tsl = lambda ap, s=s, gi=gi: ap[:, :, s * SEQ + gi * n:s * SEQ + (gi + 1) * n].rearrange("c p t -> p c t")
            mk.dma("sync", [lambda s_: s_.dma_start(out=hf[:], in_=tsl(hsrc))], writes=["hf"], ctr="ld")
            mk.op("scalar", lambda a: a.copy(out=hb[:], in_=hf[:]), reads=["hf"], writes=["hb"])
            for j in range(C):
                mk.pe([lambda t, c=c: t.matmul(psY[j % 2][:], lhsT=wqsb[:, c, j * 128:(j + 1) * 128], rhs=hb[:, c, :],
                                               start=(c == 0), stop=(c == C - 1)) for c in range(C)], reads=["w2sb0", "hb"], writes=[f"psY{j % 2}"])
                mk.op("scalar", lambda a: a.activation(out=aT[:, j, :], in_=psY[j % 2][:], func=AF.Copy, scale=scale),
                      reads=[f"psY{j % 2}"], writes=[f"aT{j}"])
            for h in range(NH):
                qk = [f"aT{h * dcs + dc}" for dc in range(dcs)]
                for mc in range(M // 128):
                    mk.pe([lambda t, dc=dc: t.matmul(psA[0][:, mc, :], lhsT=kT_sb[:, h * dcs + dc, s * M + mc * 128:s * M + (mc + 1) * 128],
                                                     rhs=aT[:, h * dcs + dc, :], start=(dc == 0), stop=(dc == dcs - 1)) for dc in range(dcs)],
                          reads=["kT"] + qk, writes=[f"psA0_{mc}"])
                    mk.op("scalar", lambda a: a.activation(out=expT[mc], in_=psA[0][:, mc, :], func=AF.Exp),
                          reads=[f"psA0_{mc}"], writes=[f"gsb{mc}"])
                ek = [f"gsb{mc}" for mc in range(M // 128)]
                mk.pe([lambda t, mc=mc: t.matmul(psG[:], lhsT=ones_bf[:], rhs=expT[mc], start=(mc == 0), stop=(mc == M // 128 - 1))
                       for mc in range(M // 128)], reads=ek + ["ones_bf"], writes=["psG"])
                V(lambda v: v.reciprocal(out=usb[:], in_=psG[:]), ["psG"], ["usb"])
                for dvc in range(dcs):
                    mk.pe([lambda t, mc=mc: t.matmul(psA[1][:, dvc, :], lhsT=v_sb[:, s * (M // 128) + mc, h * DH + dvc * 128:h * DH + (dvc + 1) * 128],
                                                     rhs=expT[mc], start=(mc == 0), stop=(mc == M // 128 - 1)) for mc in range(M // 128)],
                          reads=ek + ["vsb"], writes=[f"psA1_{dvc}"])
                    V(lambda v: v.tensor_tensor(out=hb[:, h * dcs + dvc, :], in0=psA[1][:, dvc, :], in1=usb[:], op=ALU.mult),
                      [f"psA1_{dvc}", "usb"], ["hb"])
            for m in range(C):
                mk.pe([lambda t, k_=k_: t.matmul(psY[m % 2][:], lhsT=wosb[:, k_, m * 128:(m + 1) * 128], rhs=hb[:, k_, :],
                                                 start=(k_ == 0), stop=(k_ == C - 1)) for k_ in range(C)], reads=["w2sb1", "hb"], writes=[f"psY{m % 2}"])
                V(lambda v: v.scalar_tensor_tensor(out=yacc[:, m, :], in0=hf[:, m, :], scalar=float(alpha), in1=psY[m % 2][:],
                                                   op0=ALU.mult, op1=ALU.add), ["hf", f"psY{m % 2}"], ["yaccall"])
            ln_group(mk, yacc, "yaccall", C, n, gT, bT, li, hf, "hf", psA[0][:, 0, :], "psA0_0", psA[0][:, 1, :], "psA0_1",
                     S["sq"], S["mean_sb"], S["rstd_sb"])
            mk.dma("sync", [lambda s_: s_.dma_start(out=tsl(hdst), in_=hf[:])], reads=["hf"], ctr="st")


CN = 32


def hgrn2_consts(mk, n=512):
    mk.Mk = mk.sb("Mk", [128, 4, CN], F32)
    mk.mreset = mk.sb("mreset", [128, n], F32)
    mk.onesH = mk.sb("onesH", [128, 128], F32)
    mk.op("gpsimd", lambda g: g.memset(mk.Mk[:], 1.0), writes=["Mk"])
    mk.op("gpsimd", lambda g: g.memset(mk.mreset[:], 1.0), writes=["mreset"])
    mk.op("vector", lambda v: v.memset(mk.onesH[:], 1.0 / 128), writes=["onesH"])
    for a in range(4):
        mk.op("gpsimd", lambda g, a=a: g.affine_select(out=mk.Mk[:, a, :], in_=mk.Mk[:, a, :], pattern=[[1, CN]],
                                                       compare_op=ALU.is_ge, fill=0.0, base=32 * a, channel_multiplier=-1), reads=["Mk"], writes=["Mk"])
        mk.op("gpsimd", lambda g, a=a: g.affine_select(out=mk.Mk[:, a, :], in_=mk.Mk[:, a, :], pattern=[[0, CN]],
                                                       compare_op=ALU.is_ge, fill=0.0, base=-32 * a, channel_multiplier=1), reads=["Mk"], writes=["Mk"])
    mk.op("gpsimd", lambda g: g.affine_select(out=mk.mreset[:], in_=mk.mreset[:], pattern=[[0, n // CN], [1, CN]],
                                              compare_op=ALU.not_equal, fill=0.0, base=0, channel_multiplier=0), reads=["mreset"], writes=["mreset"])


def hgrn2_phase(mk, S, H, hsrc, hdst, w_in, w_out, alb, norm_g, layer, NL, tmp, gT, bT, li, alpha, C, NSEQ, SEQ, n=512):
    nc = mk.nc
    D = C * 128
    NHD = C
    hf, hb, yacc, aT, usb = S["hf"], S["hb"], S["yacc"], S["aT"], S["usb"]
    psA, psG, psY, psM = S["psA"], S["psG"], S["psY"], S["psM"]
    wA, wB, wO = S["w1sb"][0], S["w1sb"][1], S["w2sb"][0]
    sq = S["w2sb"][1][:].bitcast(F32)
    t1, t2, t3, t4, t5, t6 = S["gsb"][0], S["gsb"][1], S["ssb"][0], S["ssb"][1], S["lsb"][0], S["lsb"][1]
    Ssb, Sbf, kk_tm, qb, kd, kkT, At = H["Ssb"], H["Sbf"], H["kk_tm"], H["qb"], H["kd"], H["kkT"], H["At"]
    lbT, omlbT, ngT, albT = H["lbT"], H["omlbT"], H["ngT"], H["albT"]
    v_tm = aT[:].rearrange("p (x y) b -> p x (y b)", y=2)
    H["on"] = aT
    psM_bf = psM[:].bitcast(BF16)
    V = lambda fn, r, w: mk.op("vector", fn, reads=r, writes=w)
    A = lambda fn, r, w: mk.op("scalar", fn, reads=r, writes=w)
    mk.dma("gpsimd", [lambda g: g.dma_start(out=wA[:], in_=w_in[:, 0:2 * D].rearrange("(c p) f -> p c f", p=128)),
                      lambda g: g.dma_start(out=wB[:], in_=w_in[:, 2 * D:4 * D].rearrange("(c p) f -> p c f", p=128)),
                      lambda g: g.dma_start(out=wO[:], in_=w_out.rearrange("(c p) f -> p c f", p=128))],
           writes=["w1sb0", "w1sb1", "w2sb0"], ctr="sw")
    load_rows_T(mk, alb, NL, D, albT, "albT", tmp, psM, "psM")
    load_rows_T(mk, norm_g, 1, D, ngT, "ngT", tmp, psM, "psM")
    A(lambda a: a.activation(out=albT[:], in_=albT[:], func=AF.Exp), ["albT"], ["albT"])
    V(lambda v: v.reduce_sum(out=omlbT[:], in_=albT[:], axis=AX.X), ["albT"], ["omlbT"])
    V(lambda v: v.reciprocal(out=omlbT[:], in_=omlbT[:]), ["omlbT"], ["omlbT"])
    if layer == 0:
        V(lambda v: v.memset(lbT[:], 0.0), [], ["lbT"])
    else:
        V(lambda v: v.reduce_sum(out=lbT[:], in_=albT[:, :, 1:layer + 1], axis=AX.X), ["albT"], ["lbT"])
        V(lambda v: v.tensor_tensor(out=lbT[:], in0=lbT[:], in1=omlbT[:], op=ALU.mult), ["lbT", "omlbT"], ["lbT"])
    V(lambda v: v.tensor_scalar(out=omlbT[:], in0=lbT[:], scalar1=-1.0, scalar2=1.0, op0=ALU.mult, op1=ALU.add), ["lbT"], ["omlbT"])
    mk.reset_all()
    for s in range(NSEQ):
        V(lambda v: v.memset(Ssb[:], 0.0), [], ["Ssb"])
        A(lambda a: a.copy(out=Sbf[:], in_=Ssb[:]), ["Ssb"], ["Sbf"])
        for gi in range(SEQ // n):
            tsl = lambda ap, s=s, gi=gi: ap[:, :, s * SEQ + gi * n:s * SEQ + (gi + 1) * n].rearrange("c p t -> p c t")
            mk.dma("sync", [lambda s_: s_.dma_start(out=hf[:], in_=tsl(hsrc))], writes=["hf"], ctr="ld")
            A(lambda a: a.copy(out=hb[:], in_=hf[:]), ["hf"], ["hb"])
            i = 0
            for tt in range(n // 128):
                for half in range(D // 512):
                    mk.pe([lambda t, c=c: t.matmul(psY[i % 2][:], lhsT=hb[:, c, tt * 128:(tt + 1) * 128], rhs=wB[:, c, half * 512:(half + 1) * 512],
                                                   start=(c == 0), stop=(c == C - 1)) for c in range(C)], reads=["w1sb1", "hb"], writes=[f"psY{i % 2}"])
                    A(lambda a: a.copy(out=v_tm[:, tt, half * 512:(half + 1) * 512], in_=psY[i % 2][:]), [f"psY{i % 2}"], ["vtm"])
                    i += 1
            for h in range(NHD):
                mk.pe([lambda t, c=c: t.matmul(psA[0][:, 0, :], lhsT=wA[:, c, D + h * 128:D + (h + 1) * 128], rhs=hb[:, c, :],
                                               start=(c == 0), stop=(c == C - 1)) for c in range(C)], reads=["w1sb0", "hb"], writes=["psF"])
                mk.pe([lambda t, c=c: t.matmul(psA[0][:, 1, :], lhsT=wA[:, c, h * 128:(h + 1) * 128], rhs=hb[:, c, :],
                                               start=(c == 0), stop=(c == C - 1)) for c in range(C)], reads=["w1sb0", "hb"], writes=["psQ"])
                A(lambda a: a.activation(out=t1[:], in_=psA[0][:, 0, :], func=AF.Sigmoid), ["psF"], ["t1"])
                V(lambda v: v.tensor_scalar(out=t1[:], in0=t1[:], scalar1=omlbT[:, h:h + 1], scalar2=lbT[:, h:h + 1], op0=ALU.mult, op1=ALU.add),
                  ["t1", "omlbT", "lbT"], ["t1"])
                V(lambda v: v.tensor_scalar(out=t2[:], in0=t1[:], scalar1=-1.0, scalar2=1.0, op0=ALU.mult, op1=ALU.add), ["t1"], ["t2"])
                V(lambda v: v.tensor_scalar(out=t1[:], in0=t1[:], scalar1=1e-6, scalar2=None, op0=ALU.max), ["t1"], ["t1"])
                A(lambda a: a.activation(out=t1[:], in_=t1[:], func=AF.Ln), ["t1"], ["t1"])
                V(lambda v: v.tensor_tensor_scan(out=t3[:], data0=mk.mreset[:], data1=t1[:], initial=0.0, op0=ALU.mult, op1=ALU.add),
                  ["t1", "mreset"], ["t3"])
                A(lambda a: a.activation(out=t4[:], in_=t3[:], func=AF.Exp), ["t3"], ["t4"])
                A(lambda a: a.activation(out=t5[:], in_=t3[:], func=AF.Exp, scale=-1.0), ["t3"], ["t5"])
                A(lambda a: a.activation(out=t6[:], in_=psA[0][:, 1, :], func=AF.Silu), ["psQ"], ["t6"])
                V(lambda v: v.tensor_tensor(out=qb[:], in0=t6[:], in1=t4[:], op=ALU.mult), ["t6", "t4"], ["qb"])
                V(lambda v: v.tensor_tensor(out=t2[:], in0=t2[:], in1=t5[:], op=ALU.mult), ["t2", "t5"], ["t2"])
                V(lambda v: v.tensor_copy(out=kd[:], in_=t2[:]), ["t2"], ["kd"])
                for c_ in range(n // CN):
                    cs = slice(c_ * CN, (c_ + 1) * CN)
                    V(lambda v: v.tensor_scalar(out=kkT[:, cs], in0=t2[:, cs], scalar1=t4[:, c_ * CN + CN - 1:c_ * CN + CN], scalar2=None, op0=ALU.mult),
                      ["t2", "t4"], ["kkT"])
                for tt in range(n // 128):
                    mk.pe([lambda t: t.transpose(out=psM_bf[:, tt * 128:(tt + 1) * 128], in_=kkT[:, tt * 128:(tt + 1) * 128], identity=H["ident_bf"][:])],
                          reads=["kkT", "ident_bf"], writes=["psM"])
                    A(lambda a: a.copy(out=kk_tm[:, tt, :], in_=psM_bf[:, tt * 128:(tt + 1) * 128]), ["psM"], ["kktm"])
                for c_ in range(n // CN):
                    cs = slice(c_ * CN, (c_ + 1) * CN)
                    tt, po = c_ // 4, (c_ % 4) * 32
                    mk.pe([lambda t: t.matmul(psG[:, 0:CN], lhsT=kd[:, tt * 128:(tt + 1) * 128], rhs=qb[:, cs], start=True, stop=True)],
                          reads=["kd", "qb"], writes=["psG"])
                    V(lambda v: v.tensor_tensor(out=At[:], in0=psG[:, 0:CN], in1=mk.Mk[:, c_ % 4, :], op=ALU.mult), ["psG", "Mk"], ["At"])
                    mk.pe([lambda t: t.matmul(psA[1][:, 1, cs], lhsT=Sbf[:, h, :], rhs=qb[:, cs], start=True, stop=False),
                           lambda t: t.matmul(psA[1][:, 1, cs], lhsT=v_tm[:, tt, h * 128:(h + 1) * 128], rhs=At[:],
                                              start=False, stop=True)], reads=["Sbf", "qb", "vtm", "At"], writes=["psO"])
                    V(lambda v: v.tensor_scalar(out=H["kkm"][:], in0=kk_tm[:, tt, :], scalar1=mk.Mk[:, c_ % 4, CN - 1:CN], scalar2=None, op0=ALU.mult),
                      ["kktm", "Mk"], ["kkm"])
                    mk.pe([lambda t: t.matmul(psM[:, 256:384], lhsT=H["kkm"][:], rhs=v_tm[:, tt, h * 128:(h + 1) * 128],
                                              start=True, stop=True)], reads=["kkm", "vtm"], writes=["psM"])
                    V(lambda v: v.scalar_tensor_tensor(out=Ssb[:, h, :], in0=Ssb[:, h, :], scalar=t4[:, c_ * CN + CN - 1:c_ * CN + CN],
                                                       in1=psM[:, 256:384], op0=ALU.mult, op1=ALU.add), ["Ssb", "t4", "psM"], ["Ssb"])
                    A(lambda a: a.copy(out=Sbf[:, h, :], in_=Ssb[:, h, :]), ["Ssb"], ["Sbf"])
                V(lambda v: v.tensor_copy(out=yacc[:, h, :], in_=psA[1][:, 1, :]), ["psO"], ["osb"])
            A(lambda a: a.activation(out=sq, in_=yacc[:], func=AF.Square), ["osb"], ["sq", "w2sb1"])
            for h in range(NHD):
                mk.pe([lambda t: t.matmul(psG[:], lhsT=mk.onesH[:], rhs=sq[:, h, :], start=True, stop=True)], reads=["sq", "w2sb1", "onesH"],
                      writes=["psG"])
                mk.pe([lambda t, c=c: t.matmul(psA[1][:, 0, :], lhsT=wB[:, c, D + h * 128:D + (h + 1) * 128], rhs=hb[:, c, :],
                                               start=(c == 0), stop=(c == C - 1)) for c in range(C)], reads=["w1sb1", "hb"], writes=["psGt"])
                V(lambda v: v.tensor_scalar(out=t1[:], in0=psG[:], scalar1=1e-5, scalar2=None, op0=ALU.add), ["psG"], ["t1"])
                A(lambda a: a.activation(out=t1[:], in_=t1[:], func=AF.Sqrt), ["t1"], ["t1"])
                V(lambda v: v.reciprocal(out=t1[:], in_=t1[:]), ["t1"], ["t1"])
                A(lambda a: a.activation(out=t6[:], in_=psA[1][:, 0, :], func=AF.Silu), ["psGt"], ["t6"])
                V(lambda v: v.tensor_tensor(out=t1[:], in0=yacc[:, h, :], in1=t1[:], op=ALU.mult), ["osb", "t1"], ["t1"])
                V(lambda v: v.scalar_tensor_tensor(out=H["on"][:, h, :], in0=t1[:], scalar=ngT[:, h, 0:1], in1=t6[:], op0=ALU.mult, op1=ALU.mult),
                  ["t1", "t6", "ngT"], ["on", "vtm"])
            for m in range(C):
                mk.pe([lambda t, k_=k_: t.matmul(psY[m % 2][:], lhsT=wO[:, k_, m * 128:(m + 1) * 128], rhs=H["on"][:, k_, :],
                                                 start=(k_ == 0), stop=(k_ == C - 1)) for k_ in range(C)], reads=["w2sb0", "on", "vtm"], writes=[f"psY{m % 2}"])
                V(lambda v: v.scalar_tensor_tensor(out=yacc[:, m, :], in0=hf[:, m, :], scalar=float(alpha), in1=psY[m % 2][:],
                                                   op0=ALU.mult, op1=ALU.add), ["hf", f"psY{m % 2}"], ["yaccall", "osb"])
            ln_group(mk, yacc, "yaccall", C, n, gT, bT, li, hf, "hf", psA[0][:, 0, :], "psF", psA[0][:, 1, :], "psQ",
                     sq, S["mean_sb"], S["rstd_sb"], sqkey="w2sb1")
            mk.dma("sync", [lambda s_: s_.dma_start(out=tsl(hdst), in_=hf[:])], reads=["hf"], ctr="st")


def hgrn2_alloc(mk, C, NL, n=512):
    H = {}
    H["Ssb"] = mk.sb("Ssb", [128, C, 128], F32)
    H["Sbf"] = mk.sb("Sbf", [128, C, 128], BF16)
    H["kk_tm"] = mk.sb("kk_tm", [128, n // 128, 128], BF16)
    H["qb"] = mk.sb("qb", [128, n], BF16)
    H["kd"] = mk.sb("kd", [128, n], BF16)
    H["kkT"] = mk.sb("kkT", [128, n], BF16)
    H["At"] = mk.sb("At", [128, CN], BF16)
    H["kkm"] = mk.sb("kkm", [128, 128], BF16)
    H["lbT"] = mk.sb("lbT", [128, C], F32)
    H["omlbT"] = mk.sb("omlbT", [128, C], F32)
    H["ngT"] = mk.sb("ngT", [128, C, 1], F32)
    H["albT"] = mk.sb("albT", [128, C, NL], F32)
    H["ident_bf"] = mk.sb("ident_bf", [128, 128], BF16)
    mk.op("vector", lambda v: v.tensor_copy(out=H["ident_bf"][:], in_=mk.ident[:]), reads=["ident"], writes=["ident_bf"])
    return H


TWO_PI = 6.283185307179586
SIN_SCALE = 6.283185


def s5_sincos(mk, u, uk, itmp, ftmp, out_sin, osk, out_cos, ock, shape_key=""):
    V = lambda fn, r, w: mk.op("vector", fn, reads=r, writes=w)
    for (shift, dst, dk) in ((0.0, out_sin, osk), (0.25, out_cos, ock)):
        V(lambda v: v.tensor_scalar(out=ftmp, in0=u, scalar1=shift, scalar2=None, op0=ALU.add), [uk], ["ftmp" + shape_key])
        V(lambda v: v.tensor_copy(out=itmp, in_=ftmp), ["ftmp" + shape_key], ["itmp" + shape_key])
        V(lambda v: v.tensor_copy(out=dst, in_=itmp), ["itmp" + shape_key], [dk])
        V(lambda v: v.tensor_tensor(out=dst, in0=ftmp, in1=dst, op=ALU.subtract), ["ftmp" + shape_key, dk], [dk])
        mk.op("scalar", lambda a: a.activation(out=dst, in_=dst, func=AF.Sin, scale=SIN_SCALE), reads=[dk], writes=[dk])


def s5_phase(mk, S, hsrc, hdst, prm, bc_pad, w_glu, c_d, tmp, gT, bT, li, alpha, C, NSEQ, SEQ, n=512):
    nc = mk.nc
    D = C * 128
    hf, hb, yacc, usb = S["hf"], S["hb"], S["yacc"], S["usb"]
    psA, psG, psY, psM = S["psA"], S["psG"], S["psY"], S["psM"]
    wG = S["w1sb"][1]
    W = S["w1sb"][0][:].bitcast(F32)
    wt = lambda i: W[:, i // 2, (i % 2) * 512:(i % 2) * 512 + n]
    cosT, sinT, bur, bui, cr, ci, zr, zi, rt, ft, tq = (wt(i) for i in range(11))
    matf = S["w2sb"][0][:].bitcast(F32)[:, 0, 0:512]
    tvals = wt(11)
    itmp = wt(12).bitcast(mybir.dt.int32)
    pc = wt(13).rearrange("p (a b) -> p a b", a=16)
    mats = wt(14).bitcast(BF16)[:, 0:512].rearrange("p (a b) -> p a b", a=4)
    carry = wt(15)[:, 0:64].rearrange("p (a b) -> p a b", b=2)
    dT = wt(15)[:, 64:64 + C].rearrange("p (a b) -> p a b", b=1)
    xr = S["aT"][:, 0, :]
    nxi = S["aT"][:, 1, :]
    ubf = S["aT"][:, 2, :]
    V = lambda fn, r, w: mk.op("vector", fn, reads=r, writes=w)
    A = lambda fn, r, w: mk.op("scalar", fn, reads=r, writes=w)
    AR, AI, LDT, DTc, MAG, THN, SN, CS, FR, FI, T0, T1, T2 = range(13)
    col = lambda i: pc[:, i, :]
    mk.reset_all()
    mk.op("gpsimd", lambda g: g.iota(out=tvals, pattern=[[1, n]], base=0, channel_multiplier=0, allow_small_or_imprecise_dtypes=True),
          writes=["tvals"])
    mk.dma("gpsimd", [lambda g: g.dma_start(out=wG[:], in_=w_glu.rearrange("(c p) f -> p c f", p=128))], writes=["w1sb1"], ctr="sw")
    load_rows_T(mk, c_d, 1, D, dT, "dT", tmp, psM, "psM")
    for i in range(3):
        mk.dma("sync", [lambda s_, i=i: s_.dma_start(out=tmp[0:32, 0:128], in_=prm[i])], writes=["tmprows"], ctr="tmprows")
        mk.pe([lambda t: t.transpose(out=psM[0:128, 0:32], in_=tmp[0:32, 0:128], identity=mk.ident[0:32, 0:32])], reads=["tmprows", "ident"], writes=["psM"])
        V(lambda v, i=i: v.tensor_copy(out=col(i), in_=psM[0:128, 0:32]), ["psM"], ["pc"])
    A(lambda a: a.activation(out=col(DTc), in_=col(LDT), func=AF.Exp), ["pc"], ["pc"])
    V(lambda v: v.tensor_tensor(out=col(T0), in0=col(AR), in1=col(DTc), op=ALU.mult), ["pc"], ["pc"])
    A(lambda a: a.activation(out=col(MAG), in_=col(T0), func=AF.Exp), ["pc"], ["pc"])
    V(lambda v: v.tensor_tensor(out=col(THN), in0=col(AI), in1=col(DTc), op=ALU.mult), ["pc"], ["pc"])
    V(lambda v: v.tensor_scalar(out=col(THN), in0=col(THN), scalar1=1.0 / TWO_PI, scalar2=None, op0=ALU.mult), ["pc"], ["pc"])
    s5_sincos(mk, col(THN), "pc", itmp[:, 0:32], col(T2), col(SN), "pc", col(CS), "pc", shape_key="c")
    V(lambda v: v.tensor_tensor(out=col(SN), in0=col(SN), in1=col(MAG), op=ALU.mult), ["pc"], ["pc"])
    V(lambda v: v.tensor_tensor(out=col(CS), in0=col(CS), in1=col(MAG), op=ALU.mult), ["pc"], ["pc"])
    V(lambda v: v.tensor_scalar(out=col(CS), in0=col(CS), scalar1=-1.0, scalar2=None, op0=ALU.add), ["pc"], ["pc"])
    V(lambda v: v.tensor_tensor(out=col(T0), in0=col(AR), in1=col(AR), op=ALU.mult), ["pc"], ["pc"])
    V(lambda v: v.tensor_tensor(out=col(T1), in0=col(AI), in1=col(AI), op=ALU.mult), ["pc"], ["pc"])
    V(lambda v: v.tensor_tensor(out=col(T0), in0=col(T0), in1=col(T1), op=ALU.add), ["pc"], ["pc"])
    V(lambda v: v.reciprocal(out=col(T0), in_=col(T0)), ["pc"], ["pc"])
    V(lambda v: v.tensor_tensor(out=col(FR), in0=col(CS), in1=col(AR), op=ALU.mult), ["pc"], ["pc"])
    V(lambda v: v.tensor_tensor(out=col(T1), in0=col(SN), in1=col(AI), op=ALU.mult), ["pc"], ["pc"])
    V(lambda v: v.tensor_tensor(out=col(FR), in0=col(FR), in1=col(T1), op=ALU.add), ["pc"], ["pc"])
    V(lambda v: v.tensor_tensor(out=col(FR), in0=col(FR), in1=col(T0), op=ALU.mult), ["pc"], ["pc"])
    V(lambda v: v.tensor_tensor(out=col(FI), in0=col(SN), in1=col(AR), op=ALU.mult), ["pc"], ["pc"])
    V(lambda v: v.tensor_tensor(out=col(T1), in0=col(CS), in1=col(AI), op=ALU.mult), ["pc"], ["pc"])
    V(lambda v: v.tensor_tensor(out=col(FI), in0=col(FI), in1=col(T1), op=ALU.subtract), ["pc"], ["pc"])
    V(lambda v: v.tensor_tensor(out=col(FI), in0=col(FI), in1=col(T0), op=ALU.mult), ["pc"], ["pc"])
    mk.reset_all()
    for s in range(NSEQ):
        V(lambda v: v.memset(carry, 0.0), [], ["carry"])
        for gi in range(SEQ // n):
            tsl = lambda ap, s=s, gi=gi: ap[:, :, s * SEQ + gi * n:s * SEQ + (gi + 1) * n].rearrange("c p t -> p c t")
            mk.dma("sync", [lambda s_: s_.dma_start(out=hf[:], in_=tsl(hsrc))], writes=["hf"], ctr="ld")
            for fc in range(C):
                A(lambda a: a.copy(out=ubf, in_=hf[:, fc, :]), ["hf"], ["ubf"])
                for q4 in range(4):
                    tau = fc * 4 + q4
                    mk.dma("sync", [lambda s_: s_.dma_start(out=matf.rearrange("p (a b) -> p a b", a=4), in_=bc_pad[tau].rearrange("a p m -> p a m"))],
                           writes=["matf", "w2sb0"], ctr="params")
                    A(lambda a: a.copy(out=mats, in_=matf.rearrange("p (a b) -> p a b", a=4)), ["matf"], ["mats"])
                    mk.pe([lambda t: t.matmul(psG[:], lhsT=mats[:, 0, :], rhs=ubf, start=True, stop=True)], reads=["mats", "ubf"], writes=["psG"])
                    mk.pe([lambda t: t.matmul(psM[:], lhsT=mats[:, 1, :], rhs=ubf, start=True, stop=True)], reads=["mats", "ubf"], writes=["psM"])
                    fr, fi = pc[:, FR, tau:tau + 1], pc[:, FI, tau:tau + 1]
                    V(lambda v: v.tensor_scalar(out=tq, in0=psM[:], scalar1=fi, scalar2=None, op0=ALU.mult), ["psM", "pc"], ["tq"])
                    V(lambda v: v.scalar_tensor_tensor(out=bur, in0=psG[:], scalar=fr, in1=tq, op0=ALU.mult, op1=ALU.subtract), ["psG", "tq"], ["bur"])
                    V(lambda v: v.tensor_scalar(out=tq, in0=psG[:], scalar1=fi, scalar2=None, op0=ALU.mult), ["psG", "pc"], ["tq"])
                    V(lambda v: v.scalar_tensor_tensor(out=bui, in0=psM[:], scalar=fr, in1=tq, op0=ALU.mult, op1=ALU.add), ["psM", "tq"], ["bui"])
                    V(lambda v: v.tensor_scalar(out=ft, in0=tvals, scalar1=float(gi * n), scalar2=pc[:, THN, tau:tau + 1], op0=ALU.add, op1=ALU.mult),
                      ["tvals", "pc"], ["ft"])
                    s5_sincos(mk, ft, "ft", itmp, tq, sinT, "sinT", cosT, "cosT")
                    V(lambda v: v.tensor_tensor(out=tq, in0=bui, in1=sinT, op=ALU.mult), ["bui", "sinT"], ["tq"])
                    V(lambda v: v.tensor_tensor(out=cr, in0=bur, in1=cosT, op=ALU.mult), ["bur", "cosT"], ["cr"])
                    V(lambda v: v.tensor_tensor(out=cr, in0=cr, in1=tq, op=ALU.add), ["cr", "tq"], ["cr"])
                    V(lambda v: v.tensor_tensor(out=tq, in0=bur, in1=sinT, op=ALU.mult), ["bur", "sinT"], ["tq"])
                    V(lambda v: v.tensor_tensor(out=ci, in0=bui, in1=cosT, op=ALU.mult), ["bui", "cosT"], ["ci"])
                    V(lambda v: v.tensor_tensor(out=ci, in0=ci, in1=tq, op=ALU.subtract), ["ci", "tq"], ["ci"])
                    V(lambda v: v.tensor_scalar(out=rt, in0=tvals, scalar1=0.0, scalar2=pc[:, MAG, tau:tau + 1], op0=ALU.mult, op1=ALU.add),
                      ["tvals", "pc"], ["rt"])
                    V(lambda v: v.tensor_tensor_scan(out=zr, data0=rt, data1=cr, initial=carry[:, tau, 0:1], op0=ALU.mult, op1=ALU.add),
                      ["rt", "cr", "carry"], ["zr"])
                    V(lambda v: v.tensor_tensor_scan(out=zi, data0=rt, data1=ci, initial=carry[:, tau, 1:2], op0=ALU.mult, op1=ALU.add),
                      ["rt", "ci", "carry"], ["zi"])
                    V(lambda v: v.tensor_copy(out=carry[:, tau, 0:1], in_=zr[:, n - 1:n]), ["zr"], ["carry"])
                    V(lambda v: v.tensor_copy(out=carry[:, tau, 1:2], in_=zi[:, n - 1:n]), ["zi"], ["carry"])
                    V(lambda v: v.tensor_tensor(out=tq, in0=zi, in1=sinT, op=ALU.mult), ["zi", "sinT"], ["tq"])
                    V(lambda v: v.tensor_tensor(out=cr, in0=zr, in1=cosT, op=ALU.mult), ["zr", "cosT"], ["cr"])
                    V(lambda v: v.tensor_tensor(out=xr, in0=cr, in1=tq, op=ALU.subtract), ["cr", "tq"], ["xr"])
                    V(lambda v: v.tensor_tensor(out=tq, in0=zr, in1=sinT, op=ALU.mult), ["zr", "sinT"], ["tq"])
                    V(lambda v: v.tensor_tensor(out=ci, in0=zi, in1=cosT, op=ALU.mult), ["zi", "cosT"], ["ci"])
                    V(lambda v: v.scalar_tensor_tensor(out=nxi, in0=ci, scalar=-1.0, in1=tq, op0=ALU.mult, op1=ALU.subtract), ["ci", "tq"], ["nxi"])
                    mk.pe([lambda t: t.matmul(psA[0][:, 0, :], lhsT=mats[:, 2, :], rhs=xr, start=(q4 == 0), stop=False),
                           lambda t: t.matmul(psA[0][:, 0, :], lhsT=mats[:, 3, :], rhs=nxi, start=False, stop=(q4 == 3))],
                          reads=["mats", "xr", "nxi"], writes=["psYs"])
                V(lambda v: v.scalar_tensor_tensor(out=bur, in0=hf[:, fc, :], scalar=dT[:, fc, 0:1], in1=psA[0][:, 0, :], op0=ALU.mult, op1=ALU.add),
                  ["hf", "dT", "psYs"], ["bur"])
                V(lambda v: v.tensor_tensor(out=bui, in0=bur, in1=bur, op=ALU.mult), ["bur"], ["bui"])
                V(lambda v: v.tensor_scalar(out=bui, in0=bui, scalar1=0.044715, scalar2=1.0, op0=ALU.mult, op1=ALU.add), ["bui"], ["bui"])
                V(lambda v: v.tensor_tensor(out=bui, in0=bui, in1=bur, op=ALU.mult), ["bui", "bur"], ["bui"])
                A(lambda a: a.activation(out=bui, in_=bui, func=AF.Tanh, scale=0.7978845608028654), ["bui"], ["bui"])
                V(lambda v: v.tensor_scalar(out=bui, in0=bui, scalar1=1.0, scalar2=0.5, op0=ALU.add, op1=ALU.mult), ["bui"], ["bui"])
                V(lambda v: v.tensor_tensor(out=hb[:, fc, :], in0=bui, in1=bur, op=ALU.mult), ["bui", "bur"], ["hb"])
            for m in range(C):
                mk.pe([lambda t, c=c: t.matmul(psY[0][:], lhsT=wG[:, c, m * 128:(m + 1) * 128], rhs=hb[:, c, :], start=(c == 0), stop=(c == C - 1))
                       for c in range(C)], reads=["w1sb1", "hb"], writes=["psY0"])
                mk.pe([lambda t, c=c: t.matmul(psY[1][:], lhsT=wG[:, c, D + m * 128:D + (m + 1) * 128], rhs=hb[:, c, :], start=(c == 0), stop=(c == C - 1))
                       for c in range(C)], reads=["w1sb1", "hb"], writes=["psY1"])
                A(lambda a: a.activation(out=usb[:], in_=psY[1][:], func=AF.Sigmoid), ["psY1"], ["usb"])
                V(lambda v: v.tensor_tensor(out=usb[:], in0=psY[0][:], in1=usb[:], op=ALU.mult), ["psY0", "usb"], ["usb"])
                V(lambda v: v.scalar_tensor_tensor(out=yacc[:, m, :], in0=hf[:, m, :], scalar=float(alpha), in1=usb[:], op0=ALU.mult, op1=ALU.add),
                  ["hf", "usb"], ["yaccall"])
            ln_group(mk, yacc, "yaccall", C, n, gT, bT, li, hf, "hf", psA[1][:, 0, :], "psA1a", psA[1][:, 1, :], "psA1b",
                     S["w2sb"][1][:].bitcast(F32), S["mean_sb"], S["rstd_sb"], sqkey="w2sb1")
            mk.dma("sync", [lambda s_: s_.dma_start(out=tsl(hdst), in_=hf[:])], reads=["hf"], ctr="st")


DIL_GROUPS = ((128, 1), (512, 4), (2048, 16))
I32 = mybir.dt.int32


def dil_phase(mk, S, hsrc, hdst, attn_d, w_in, w_out, posrow, gT, bT, li, alpha, C, NSEQ, SEQ, n=512):
    nc = mk.nc
    D = C * 128
    NT = SEQ // 128
    hf, hb, yacc, aT, usb = S["hf"], S["hb"], S["yacc"], S["aT"], S["usb"]
    psA, psG, psY, psM = S["psA"], S["psG"], S["psY"], S["psM"]
    A0 = S["w1sb"][0][:].rearrange("p c f -> p (c f)")
    A1 = S["w1sb"][1][:].rearrange("p c f -> p (c f)")
    qT = [A0[:, g * SEQ:(g + 1) * SEQ] for g in range(3)]
    kT = [A1[:, g * SEQ:(g + 1) * SEQ] for g in range(3)]
    AX_ = aT[:].rearrange("p a b -> p (a b)")
    vflat = [A0[:, 3 * SEQ:4 * SEQ], A1[:, 3 * SEQ:4 * SEQ], AX_[:, 0:SEQ]]
    v_tm = [v.rearrange("p (t d) -> p t d", d=128) for v in vflat]
    wsl = S["w2sb"][0][:].rearrange("p c f -> p (c f)")[:, 0:C * 384].rearrange("p (c f) -> p c f", f=384)
    attn_sb = hb[:].rearrange("p c f -> p (c f)")[:, 0:SEQ]
    YB = yacc[:].rearrange("p c f -> p (c f)").bitcast(BF16)
    masks = YB[:, 0:24 * 128].rearrange("p (m q) -> p m q", q=128)
    Pt = YB[:, 24 * 128:25 * 128]
    t1, t2, t3, t4, t5, t6 = S["gsb"][0], S["gsb"][1], S["ssb"][0], S["ssb"][1], S["lsb"][0], S["lsb"][1]
    cosT, sinT = S["mean_sb"], S["rstd_sb"]
    ms = S["lg"]
    mi = S["msk"][:].bitcast(I32)
    Rm, rlo, rhi, invf = S["ex"], S["ssum"][:, 0:1], S["ssum"][:, 1:2], S["ssum"][:, 2:3]
    V = lambda fn, r, w: mk.op("vector", fn, reads=r, writes=w)
    A = lambda fn, r, w: mk.op("scalar", fn, reads=r, writes=w)
    G = lambda fn, r, w: mk.op("gpsimd", fn, reads=r, writes=w)
    mk.reset_all()
    G(lambda g: g.iota(out=mi[:, 0:1], pattern=[[0, 1]], base=0, channel_multiplier=1), [], ["mi"])
    V(lambda v: v.tensor_single_scalar(out=mi[:, 1:2], in_=mi[:, 0:1], scalar=31, op=ALU.bitwise_and), ["mi"], ["mi"])
    V(lambda v: v.tensor_single_scalar(out=mi[:, 2:3], in_=mi[:, 0:1], scalar=32, op=ALU.bitwise_and), ["mi"], ["mi"])
    V(lambda v: v.tensor_copy(out=ms[:, 0:2], in_=mi[:, 1:3]), ["mi"], ["ms"])
    A(lambda a_: a_.activation(out=invf, in_=ms[:, 0:1], func=AF.Exp, scale=-math.log(10000.0) / 32.0), ["ms"], ["ssum"])
    V(lambda v: v.tensor_scalar(out=invf, in0=invf, scalar1=1.0 / TWO_PI, scalar2=None, op0=ALU.mult), ["ssum"], ["ssum"])
    V(lambda v: v.tensor_scalar(out=rlo, in0=ms[:, 1:2], scalar1=0.0, scalar2=None, op0=ALU.is_equal), ["ms"], ["ssum"])
    V(lambda v: v.tensor_scalar(out=rhi, in0=rlo, scalar1=-1.0, scalar2=1.0, op0=ALU.mult, op1=ALU.add), ["ssum"], ["ssum"])
    G(lambda g: g.memset(Rm[:], 1.0), [], ["Rm"])
    G(lambda g: g.memset(ms[:], 1.0), ["ms"], ["ms"])
    G(lambda g: g.affine_select(out=Rm[:], in_=Rm[:], pattern=[[-1, 128]], compare_op=ALU.is_equal, fill=0.0, base=32, channel_multiplier=1), ["Rm"], ["Rm"])
    G(lambda g: g.affine_select(out=ms[:], in_=ms[:], pattern=[[-1, 128]], compare_op=ALU.is_equal, fill=0.0, base=-32, channel_multiplier=1), ["ms"], ["ms"])
    V(lambda v: v.tensor_scalar(out=Rm[:], in0=Rm[:], scalar1=rlo, scalar2=None, op0=ALU.mult), ["Rm", "ssum"], ["Rm"])
    V(lambda v: v.tensor_scalar(out=ms[:], in0=ms[:], scalar1=rhi, scalar2=None, op0=ALU.mult), ["ms", "ssum"], ["ms"])
    V(lambda v: v.tensor_tensor(out=Rm[:], in0=Rm[:], in1=ms[:], op=ALU.subtract), ["Rm", "ms"], ["Rm"])
    mlist = []
    for gidx, (win, d) in enumerate(DIL_GROUPS):
        for m in range(win // 128 + 1):
            mlist.append((gidx, d, m))
    mindex = {(g_, m_): i for i, (g_, d_, m_) in enumerate(mlist)}
    for i, (g_, d, m) in enumerate(mlist):
        G(lambda g: g.iota(out=mi, pattern=[[1, 128]], base=128 * m + 4096, channel_multiplier=-1), [], ["mi"])
        V(lambda v: v.tensor_single_scalar(out=mi, in_=mi, scalar=d - 1, op=ALU.bitwise_and), ["mi"], ["mi"])
        V(lambda v: v.tensor_copy(out=ms[:], in_=mi), ["mi"], ["ms"])
        V(lambda v: v.tensor_scalar(out=ms[:], in0=ms[:], scalar1=0.0, scalar2=None, op0=ALU.is_equal), ["ms"], ["ms"])
        G(lambda g: g.affine_select(out=ms[:], in_=ms[:], pattern=[[1, 128]], compare_op=ALU.is_ge, fill=0.0, base=128 * m, channel_multiplier=-1), ["ms"], ["ms"])
        G(lambda g: g.affine_select(out=ms[:], in_=ms[:], pattern=[[-1, 128]], compare_op=ALU.is_ge, fill=0.0, base=128 * d - 128 * m, channel_multiplier=1),
          ["ms"], ["ms"])
        V(lambda v, i=i: v.tensor_copy(out=masks[:, i, :], in_=ms[:]), ["ms"], ["masks"])
    for s in range(NSEQ):
        for hp in range(C):
            for g_ in range(3):
                base = g_ * 3 * D
                mk.dma("gpsimd", [lambda gq, j=j: gq.dma_start(out=wsl[:, :, j * 128:(j + 1) * 128],
                                                               in_=w_in[:, base + j * D + hp * 128:base + j * D + (hp + 1) * 128].rearrange("(c p) f -> p c f", p=128))
                                  for j in range(3)], writes=["wsl"], ctr="sw")
                for gi in range(SEQ // n):
                    tsl = lambda ap, s=s, gi=gi: ap[:, :, s * SEQ + gi * n:s * SEQ + (gi + 1) * n].rearrange("c p t -> p c t")
                    mk.dma("sync", [lambda s_: s_.dma_start(out=hf[:], in_=tsl(hsrc))], writes=["hf"], ctr="ld")
                    A(lambda a_: a_.copy(out=hb[:], in_=hf[:]), ["hf"], ["hb"])
                    mk.dma("sync", [lambda s_: s_.dma_start(out=S["gates"][0:1, :].bitcast(I32),
                                                            in_=posrow[s, :, gi * n:gi * n + 128])], writes=["gates"], ctr="params")
                    for q in range(n // 128):
                        if q:
                            mk.dma("sync", [lambda s_, q=q: s_.dma_start(out=S["gates"][0:1, :].bitcast(I32), in_=posrow[s, :, gi * n + q * 128:gi * n + (q + 1) * 128])],
                                   writes=["gates"], ctr="params")
                        V(lambda v: v.tensor_copy(out=S["gatesT"][0:1, 0:128], in_=S["gates"][0:1, :].bitcast(I32)), ["gates"], ["posf"])
                        mk.pe([lambda t, q=q: t.matmul(psM[:, q * 128:(q + 1) * 128], lhsT=mk.ones1[0:1, :], rhs=S["gatesT"][0:1, 0:128], start=True, stop=True)],
                              reads=["posf", "ones1"], writes=["psM"])
                    V(lambda v: v.tensor_scalar(out=t1[:], in0=psM[:], scalar1=invf, scalar2=None, op0=ALU.mult), ["psM", "ssum"], ["t1"])
                    s5_sincos(mk, t1[:], "t1", S["usb"][:].bitcast(I32), t2[:], sinT[:], "sinT", cosT[:], "cosT", shape_key="d")
                    for j in range(2):
                        mk.pe([lambda t, c=c: t.matmul(psY[j][:], lhsT=wsl[:, c, j * 128:(j + 1) * 128], rhs=hb[:, c, :], start=(c == 0), stop=(c == C - 1))
                               for c in range(C)], reads=["wsl", "hb"], writes=[f"psY{j}"])
                        A(lambda a_: a_.activation(out=t3[:], in_=psY[j][:], func=AF.Copy, scale=(0.125 if j == 0 else 1.0)), [f"psY{j}"], ["t3"])
                        mk.pe([lambda t: t.matmul(psG[:], lhsT=Rm[:], rhs=t3[:], start=True, stop=True)], reads=["Rm", "t3"], writes=["psG"])
                        V(lambda v: v.tensor_tensor(out=t4[:], in0=psG[:], in1=sinT[:], op=ALU.mult), ["psG", "sinT"], ["t4"])
                        V(lambda v: v.tensor_tensor(out=t3[:], in0=t3[:], in1=cosT[:], op=ALU.mult), ["t3", "cosT"], ["t3"])
                        dst = (qT if j == 0 else kT)[g_][:, gi * n:(gi + 1) * n]
                        V(lambda v: v.tensor_tensor(out=dst, in0=t3[:], in1=t4[:], op=ALU.add), ["t3", "t4"], ["qk"])
                    for tt in range(n // 128):
                        mk.pe([lambda t, c=c: t.matmul(psA[1][:, 0, 0:128], lhsT=hb[:, c, tt * 128:(tt + 1) * 128], rhs=wsl[:, c, 256:384],
                                                       start=(c == 0), stop=(c == C - 1)) for c in range(C)], reads=["wsl", "hb"], writes=["psV"])
                        A(lambda a_: a_.copy(out=v_tm[g_][:, gi * (n // 128) + tt, :], in_=psA[1][:, 0, 0:128]), ["psV"], ["vtm"])
            for half in range(2):
                pb = half * 64
                for qt in range(NT):
                    units = [(g_, m) for g_, (win, d) in enumerate(DIL_GROUPS) for m in range(win // 128 + 1) if qt - m >= 0]
                    for ui, (g_, m) in enumerate(units):
                        kt = qt - m
                        mk.pe([lambda t: t.matmul(psG[:, 0:128], lhsT=kT[g_][pb:pb + 64, kt * 128:(kt + 1) * 128],
                                                  rhs=qT[g_][pb:pb + 64, qt * 128:(qt + 1) * 128], start=True, stop=True)], reads=["qk"], writes=["psG"])
                        A(lambda a_: a_.activation(out=Pt, in_=psG[:, 0:128], func=AF.Exp), ["psG"], ["Pt"])
                        V(lambda v: v.tensor_tensor(out=Pt, in0=Pt, in1=masks[:, mindex[(g_, m)], :], op=ALU.mult), ["Pt", "masks"], ["Pt"])
                        mk.pe([lambda t: t.matmul(psA[0][pb:pb + 64, 0, 0:128], lhsT=v_tm[g_][:, kt, pb:pb + 64], rhs=Pt,
                                                  start=(ui == 0), stop=(ui == len(units) - 1)),
                               lambda t: t.matmul(psA[0][pb:pb + 64, 1, 0:128], lhsT=mk.ones_bf[:, 0:64], rhs=Pt,
                                                  start=(ui == 0), stop=(ui == len(units) - 1))], reads=["vtm", "Pt", "ones_bf"], writes=["psN"])
                    V(lambda v: v.reciprocal(out=t5[pb:pb + 64, 0:128], in_=psA[0][pb:pb + 64, 1, 0:128]), ["psN"], ["t5"])
                    V(lambda v: v.tensor_tensor(out=attn_sb[pb:pb + 64, qt * 128:(qt + 1) * 128], in0=psA[0][pb:pb + 64, 0, 0:128], in1=t5[pb:pb + 64, 0:128],
                                                op=ALU.mult), ["psN", "t5"], ["hb"])
            mk.dma("sync", [lambda s_: s_.dma_start(out=attn_d[hp, :, s * SEQ:(s + 1) * SEQ], in_=attn_sb)], reads=["hb"], writes=["attnd"], ctr="st")
    wO = S["w2sb"][0]
    mk.dma("gpsimd", [lambda g: g.dma_start(out=wO[:], in_=w_out.rearrange("(c p) f -> p c f", p=128))], writes=["w2sb0", "wsl"], ctr="sw")
    mk.reset_all()
    for s in range(NSEQ):
        for gi in range(SEQ // n):
            tsl = lambda ap, s=s, gi=gi: ap[:, :, s * SEQ + gi * n:s * SEQ + (gi + 1) * n].rearrange("c p t -> p c t")
            mk.dma("sync", [lambda s_: s_.dma_start(out=hf[:], in_=tsl(hsrc)), lambda s_: s_.dma_start(out=hb[:], in_=tsl(attn_d))], writes=["hf", "hb"], ctr="ld")
            for m in range(C):
                mk.pe([lambda t, k_=k_: t.matmul(psY[m % 2][:], lhsT=wO[:, k_, m * 128:(m + 1) * 128], rhs=hb[:, k_, :], start=(k_ == 0), stop=(k_ == C - 1))
                       for k_ in range(C)], reads=["w2sb0", "hb"], writes=[f"psY{m % 2}"])
                V(lambda v: v.scalar_tensor_tensor(out=yacc[:, m, :], in0=hf[:, m, :], scalar=float(alpha), in1=psY[m % 2][:], op0=ALU.mult, op1=ALU.add),
                  ["hf", f"psY{m % 2}"], ["yaccall"])
            ln_group(mk, yacc, "yaccall", C, n, gT, bT, li, hf, "hf", psA[0][:, 0, :], "psN", psA[0][:, 1, :], "psN2",
                     S["w2sb"][1][:].bitcast(F32), S["mean_sb"], S["rstd_sb"], sqkey="w2sb1")
            mk.dma("sync", [lambda s_: s_.dma_start(out=tsl(hdst), in_=hf[:])], reads=["hf"], ctr="st")
```
